# Optimizing a Trainium2 kernel written in Bass

```python
import jax, jax.numpy as jnp
from jax import lax
import numpy as np

D_MODEL = 1024
BATCH = 4
SEQ = 4096
DEPTH = 2

GRID_W = 64
CTX_LEN = 256
EPS = 1e-6
F32 = jnp.float32
NEG_INF = -1e30
F_FLOOR = 1e-30
NA_HEADS = 8
NA_HEAD_DIM = 64
NA_WIDTH = NA_HEADS * NA_HEAD_DIM
NA_WIN_ROWS = 8
NA_WIN_COLS = 16
NA_QCOL_BLOCK = 16
NA_KCOL_BAND = NA_QCOL_BLOCK + NA_WIN_COLS
HG_HEADS = 4
HG_HEAD_DIM = 128
HG_WIDTH = HG_HEADS * HG_HEAD_DIM
ML_HEADS = 4
ML_HEAD_DIM = 128
ML_WIDTH = ML_HEADS * ML_HEAD_DIM
ML_GATE_COLS = 2 * 2 * ML_HEADS
ROPE_BASE = 10000.0
CHUNK = 64
N_BRANCH = 3
BRANCH_WIDTH = 512
FFN_DIM = 2816
N_EXPERTS = 8
TOP_K = 2
EXPERT_DIM = 3584
MOE_BLOCK = 512

IN_NAMES = ('na_q', 'na_k', 'na_v', 'hg_q', 'hg_f_fwd', 'hg_f_bwd', 'hg_i', 'hg_g',
            'ml_q', 'ml_k', 'ml_v', 'ml_o', 'ml_gates', 'branch_gates')
IN_SIZES = (NA_WIDTH, NA_WIDTH, NA_WIDTH, HG_WIDTH, HG_WIDTH, HG_WIDTH, HG_WIDTH, HG_WIDTH,
            ML_WIDTH, ML_WIDTH, ML_WIDTH, ML_WIDTH, ML_GATE_COLS, N_BRANCH * D_MODEL)
IN_WIDTH = 3 * NA_WIDTH + 5 * HG_WIDTH + 4 * ML_WIDTH + ML_GATE_COLS + N_BRANCH * D_MODEL

kernel_name = 'hybrid_na_hgrn2_mlstm_moe_dit'


def rms_norm(x, w):
    xf = x.astype(F32)
    y = xf * lax.rsqrt(jnp.mean(xf * xf, axis=-1, keepdims=True) + EPS)
    return (y * w.astype(F32)).astype(x.dtype)


def modulate(h, shift, scale):
    return h * (1 + scale) + shift


def split_heads(t, n):
    return t.reshape(*t.shape[:-1], n, t.shape[-1] // n)


def split_in(proj):
    offs = np.cumsum(IN_SIZES)[:-1].tolist()
    return dict(zip(IN_NAMES, jnp.split(proj, offs, axis=-1)))


def head_rms(o, w):
    o = o * lax.rsqrt(jnp.mean(o * o, axis=-1, keepdims=True) + EPS)
    return o.reshape(*o.shape[:2], -1) * w.astype(F32)


def axial_rope(t, rows_pos, cols_pos):
    half = t.shape[-1] // 2
    n_freq = half // 2
    inv_freq = ROPE_BASE ** (-jnp.arange(n_freq, dtype=F32) / n_freq)

    def rotate(part, pos):
        ang = pos.astype(F32)[:, None] * inv_freq
        cos = jnp.cos(ang)[None, :, None, :]
        sin = jnp.sin(ang)[None, :, None, :]
        p1, p2 = part[..., :n_freq], part[..., n_freq:]
        return jnp.concatenate([p1 * cos - p2 * sin, p1 * sin + p2 * cos], axis=-1)

    return jnp.concatenate([rotate(t[..., :half], rows_pos), rotate(t[..., half:], cols_pos)], axis=-1)


def to_chunks(a):
    B, T, H, d = a.shape
    return a.reshape(B, T // CHUNK, CHUNK, H, d).transpose(1, 0, 3, 2, 4)


def gate_chunks(a):
    B, T, H = a.shape
    return a.reshape(B, T // CHUNK, CHUNK, H).transpose(1, 0, 3, 2)


def from_chunks(a):
    n, B, H, L, d = a.shape
    return a.transpose(1, 0, 3, 2, 4).reshape(B, n * L, H, d)


def na_latent(q, k, v, k_ctx, v_ctx, rpb):
    B, S, H, dh = q.shape
    rows = S // GRID_W
    wr = min(NA_WIN_ROWS, rows)
    n_cb = GRID_W // NA_QCOL_BLOCK
    scale = dh ** -0.5
    qcol = np.arange(GRID_W).reshape(n_cb, NA_QCOL_BLOCK)
    band_start = np.clip(np.arange(n_cb) * NA_QCOL_BLOCK - NA_WIN_COLS // 2, 0, GRID_W - NA_KCOL_BAND)
    kcol = band_start[:, None] + np.arange(NA_KCOL_BAND)
    win_start = np.clip(qcol - NA_WIN_COLS // 2, 0, GRID_W - NA_WIN_COLS)
    col_mask = (kcol[:, None, :] >= win_start[..., None]) & (kcol[:, None, :] < win_start[..., None] + NA_WIN_COLS)
    dc = np.clip(kcol[:, None, :] - qcol[..., None] + NA_WIN_COLS - 1, 0, 2 * NA_WIN_COLS - 2)
    rpb_c = rpb.astype(F32)[:, :, dc]
    qg = q.reshape(B, rows, GRID_W, H, dh)
    kband = k.reshape(B, rows, GRID_W, H, dh)[:, :, kcol]
    vband = v.reshape(B, rows, GRID_W, H, dh)[:, :, kcol]
    mask = jnp.asarray(col_mask)[:, :, None, :]

    def row_block(r):
        r0 = jnp.clip(r - NA_WIN_ROWS // 2, 0, rows - wr)
        q_r = lax.dynamic_index_in_dim(qg, r, axis=1, keepdims=False).reshape(B, n_cb, NA_QCOL_BLOCK, H, dh)
        k_r = lax.dynamic_slice_in_dim(kband, r0, wr, axis=1)
        v_r = lax.dynamic_slice_in_dim(vband, r0, wr, axis=1)
        s_loc = jnp.einsum('bcqhd,bwckhd->bhcqwk', q_r, k_r, preferred_element_type=F32) * scale
        dr = r0 + jnp.arange(wr) - r + NA_WIN_ROWS - 1
        bias = jnp.take(rpb_c, dr, axis=1).transpose(0, 2, 3, 1, 4)
        s_loc = jnp.where(mask, s_loc + bias, NEG_INF).reshape(B, H, n_cb, NA_QCOL_BLOCK, wr * NA_KCOL_BAND)
        s_ctx = jnp.einsum('bcqhd,bjhd->bhcqj', q_r, k_ctx, preferred_element_type=F32) * scale
        p = jax.nn.softmax(jnp.concatenate([s_loc, s_ctx], axis=-1), axis=-1).astype(v.dtype)
        p_loc = p[..., :wr * NA_KCOL_BAND].reshape(B, H, n_cb, NA_QCOL_BLOCK, wr, NA_KCOL_BAND)
        p_ctx = p[..., wr * NA_KCOL_BAND:]
        o = jnp.einsum('bhcqwk,bwckhd->bcqhd', p_loc, v_r) + jnp.einsum('bhcqj,bjhd->bcqhd', p_ctx, v_ctx)
        return o.reshape(B, GRID_W, H, dh)

    out = lax.map(row_block, jnp.arange(rows))
    return out.transpose(1, 0, 2, 3, 4).reshape(B, S, H * dh)


def context_attention(q, k, v):
    B, L, H, dh = q.shape
    s = jnp.einsum('bqhd,bkhd->bhqk', q, k, preferred_element_type=F32) * dh ** -0.5
    p = jax.nn.softmax(s, axis=-1).astype(v.dtype)
    return jnp.einsum('bhqk,bkhd->bqhd', p, v).reshape(B, L, H * dh)


def bidirectional(scan_fn, ctx_dirs, lat_dirs, init):
    outs_c, outs_l = [], []
    for d in range(2):
        rev = (lambda a: a) if d == 0 else (lambda a: jnp.flip(a, axis=1))
        o_c, state = scan_fn(*[rev(a) for a in ctx_dirs[d]], init)
        o_l, _ = scan_fn(*[rev(a) for a in lat_dirs[d]], state)
        outs_c.append(rev(o_c))
        outs_l.append(rev(o_l))
    return outs_c[0] + outs_c[1], outs_l[0] + outs_l[1]


def hgrn2_chunk_scan(q, k, v, log_f, state):
    tri = jnp.tril(jnp.ones((CHUNK, CHUNK), dtype=bool))[:, :, None]

    def step(S, inp):
        qc, kc, vc, gc = inp
        G = jnp.cumsum(gc, axis=2)
        decay = jnp.exp(jnp.where(tri, G[:, :, :, None, :] - G[:, :, None, :, :], NEG_INF))
        A = jnp.einsum('bhtd,bhsd,bhtsd->bhts', qc, kc, decay)
        o = jnp.einsum('bhts,bhsv->bhtv', A, vc) + jnp.einsum('bhtd,bhdv->bhtv', qc * jnp.exp(G), S)
        G_end = G[:, :, -1, :]
        S_new = jnp.exp(G_end)[..., None] * S + jnp.einsum('bhsd,bhsv->bhdv', kc * jnp.exp(G_end[:, :, None, :] - G), vc)
        return S_new, o

    S_fin, o = lax.scan(step, state, (to_chunks(q), to_chunks(k), to_chunks(v), to_chunks(log_f)))
    return from_chunks(o), S_fin


def hgrn2_branch(p_ctx, p_lat, lower, norm_w):
    def prep(p):
        q = jax.nn.silu(split_heads(p['hg_q'], HG_HEADS).astype(F32))
        v = split_heads(p['hg_i'], HG_HEADS).astype(F32)
        per_dir = []
        for d, name in enumerate(('hg_f_fwd', 'hg_f_bwd')):
            z = split_heads(p[name], HG_HEADS).astype(F32)
            lb = lower[d].reshape(HG_HEADS, HG_HEAD_DIM)
            k = (1.0 - lb) * jax.nn.sigmoid(-z)
            f = lb + (1.0 - lb) * jax.nn.sigmoid(z)
            log_f = jnp.log(jnp.maximum(f, F_FLOOR))
            per_dir.append((q, k, v, log_f))
        return per_dir

    B = p_lat['hg_q'].shape[0]
    init = jnp.zeros((B, HG_HEADS, HG_HEAD_DIM, HG_HEAD_DIM), F32)
    o_c, o_l = bidirectional(hgrn2_chunk_scan, prep(p_ctx), prep(p_lat), init)

    def readout(o, p):
        return head_rms(o, norm_w) * jax.nn.silu(p['hg_g'].astype(F32))

    return readout(o_c, p_ctx), readout(o_l, p_lat)


def mlstm_chunk_scan(q, k, v, log_i, log_f, state):
    tri = jnp.tril(jnp.ones((CHUNK, CHUNK), dtype=bool))

    def step(carry, inp):
        C, nv, m = carry
        qc, kc, vc, ic, fc = inp
        b = jnp.cumsum(fc, axis=-1)
        dmat = jnp.where(tri, b[..., :, None] - b[..., None, :] + ic[..., None, :], NEG_INF)
        inter = b + m[..., None]
        m_t = jnp.maximum(inter, jnp.max(dmat, axis=-1))
        w_inter = jnp.exp(inter - m_t)
        p = jnp.exp(dmat - m_t[..., None]) * jnp.einsum('bhtd,bhsd->bhts', qc, kc)
        num = jnp.einsum('bhts,bhsv->bhtv', p, vc) + w_inter[..., None] * jnp.einsum('bhtd,bhdv->bhtv', qc, C)
        den = jnp.sum(p, axis=-1) + w_inter * jnp.einsum('bhtd,bhd->bht', qc, nv)
        h = num / jnp.maximum(jnp.abs(den), jnp.exp(-m_t))[..., None]
        b_end = b[..., -1]
        e = b_end[..., None] - b + ic
        m_new = jnp.maximum(b_end + m, jnp.max(e, axis=-1))
        w_old = jnp.exp(b_end + m - m_new)
        w_s = jnp.exp(e - m_new[..., None])
        C_new = w_old[..., None, None] * C + jnp.einsum('bhs,bhsd,bhsv->bhdv', w_s, kc, vc)
        n_new = w_old[..., None] * nv + jnp.einsum('bhs,bhsd->bhd', w_s, kc)
        return (C_new, n_new, m_new), h

    xs = (to_chunks(q), to_chunks(k), to_chunks(v), gate_chunks(log_i), gate_chunks(log_f))
    state_out, h = lax.scan(step, state, xs)
    return from_chunks(h), state_out


def mlstm_branch(p_ctx, p_lat, gate_b, norm_w, rows_pos, cols_pos):
    def prep(p, use_rope):
        q = split_heads(p['ml_q'], ML_HEADS).astype(F32)
        k = split_heads(p['ml_k'], ML_HEADS).astype(F32) * ML_HEAD_DIM ** -0.5
        v = split_heads(p['ml_v'], ML_HEADS).astype(F32)
        if use_rope:
            q = axial_rope(q, rows_pos, cols_pos)
            k = axial_rope(k, rows_pos, cols_pos)
        B, T = q.shape[:2]
        g = p['ml_gates'].astype(F32).reshape(B, T, 2, 2, ML_HEADS) + gate_b.astype(F32)
        return [(q, k, v, g[:, :, d, 0], jax.nn.log_sigmoid(g[:, :, d, 1])) for d in range(2)]

    B = p_lat['ml_q'].shape[0]
    init = (jnp.zeros((B, ML_HEADS, ML_HEAD_DIM, ML_HEAD_DIM), F32),
            jnp.zeros((B, ML_HEADS, ML_HEAD_DIM), F32),
            jnp.zeros((B, ML_HEADS), F32))
    h_c, h_l = bidirectional(mlstm_chunk_scan, prep(p_ctx, False), prep(p_lat, True), init)

    def readout(h, p):
        return jax.nn.sigmoid(p['ml_o'].astype(F32)) * head_rms(h, norm_w)

    return readout(h_c, p_ctx), readout(h_l, p_lat)


def gated_merge(branches, gate_raw, w_branch, w_out):
    dt = gate_raw.dtype
    gates = jnp.split(jax.nn.sigmoid(gate_raw.astype(F32)).astype(dt), N_BRANCH, axis=-1)
    y = gates[0] * (branches[0].astype(dt) @ w_branch[0])
    for i in range(1, N_BRANCH):
        y = y + gates[i] * (branches[i].astype(dt) @ w_branch[i])
    return y @ w_out


def hybrid_mixer(a_lat, a_ctx, w_in, na_rpb, hg_lower, hg_norm_w, ml_gate_b, ml_norm_w, w_branch, w_out,
                 rows_pos, cols_pos, need_ctx_out):
    pl = split_in(a_lat @ w_in)
    pc = split_in(a_ctx @ w_in)
    nh = lambda t: split_heads(t, NA_HEADS)
    na_l = na_latent(nh(pl['na_q']), nh(pl['na_k']), nh(pl['na_v']), nh(pc['na_k']), nh(pc['na_v']), na_rpb)
    hg_c, hg_l = hgrn2_branch(pc, pl, hg_lower, hg_norm_w)
    ml_c, ml_l = mlstm_branch(pc, pl, ml_gate_b, ml_norm_w, rows_pos, cols_pos)
    y_lat = gated_merge((na_l, hg_l, ml_l), pl['branch_gates'], w_branch, w_out)
    if not need_ctx_out:
        return y_lat, None
    na_c = context_attention(nh(pc['na_q']), nh(pc['na_k']), nh(pc['na_v']))
    y_ctx = gated_merge((na_c, hg_c, ml_c), pc['branch_gates'], w_branch, w_out)
    return y_lat, y_ctx


def swiglu(h, w_up, w_down):
    a, u = jnp.split(h @ w_up, 2, axis=-1)
    return (jax.nn.silu(a) * u) @ w_down


def moe_swiglu(h, w_router, w_up, w_down):
    B, T, D = h.shape
    x = h.reshape(B * T, D)
    n_assign = B * T * TOP_K
    logits = jnp.einsum('nd,de->ne', x, w_router, preferred_element_type=F32)
    top_v, top_e = lax.top_k(logits, TOP_K)
    top_w = jax.nn.softmax(top_v, axis=-1)
    flat_e = top_e.reshape(-1)
    flat_tok = jnp.arange(n_assign, dtype=jnp.int32) // TOP_K
    flat_w = top_w.reshape(-1)
    order = jnp.argsort(flat_e)
    e_sorted = flat_e[order]
    counts = jnp.bincount(flat_e, length=N_EXPERTS)
    padded = (counts + MOE_BLOCK - 1) // MOE_BLOCK * MOE_BLOCK
    pad_end = jnp.cumsum(padded)
    pad_start = pad_end - padded
    grp_start = jnp.cumsum(counts) - counts
    dest = pad_start[e_sorted] + jnp.arange(n_assign) - grp_start[e_sorted]
    n_blocks = -(-(n_assign + N_EXPERTS * (MOE_BLOCK - 1)) // MOE_BLOCK)
    n_slots = n_blocks * MOE_BLOCK
    slot_tok = jnp.zeros((n_slots,), jnp.int32).at[dest].set(flat_tok[order])
    slot_w = jnp.zeros((n_slots,), F32).at[dest].set(flat_w[order])
    block_e = jnp.minimum(jnp.searchsorted(pad_end, jnp.arange(n_blocks) * MOE_BLOCK, side='right'), N_EXPERTS - 1)

    def block_fn(args):
        tok, w, e = args
        a, u = jnp.split(x[tok] @ w_up[e], 2, axis=-1)
        return ((jax.nn.silu(a) * u) @ w_down[e]) * w[:, None].astype(x.dtype)

    y = lax.map(block_fn, (slot_tok.reshape(n_blocks, MOE_BLOCK), slot_w.reshape(n_blocks, MOE_BLOCK), block_e))
    out = jnp.zeros_like(x).at[slot_tok].add(y.reshape(n_slots, D))
    return out.reshape(B, T, D)


def setup_inputs(seed: int = 0) -> dict:
    key = jax.random.key(seed)
    ks = jax.random.split(key, 24)
    D = D_MODEL
    n_dense = (DEPTH + 1) // 2
    n_moe = DEPTH // 2
    nrm = lambda k, shape, s: jax.random.normal(k, shape, F32) * s
    return {
        'x': nrm(ks[0], (BATCH, SEQ, D), 1.0),
        'c': nrm(ks[1], (BATCH, D), 1.0),
        'ctx': nrm(ks[2], (BATCH, CTX_LEN, D), 1.0),
        'c_ctx': nrm(ks[3], (D,), 1.0),
        'mod_w': nrm(ks[4], (DEPTH, D, 6 * D), 0.5 * D ** -0.5),
        'mod_b': nrm(ks[5], (DEPTH, 6 * D), 0.02),
        'norm1_w': 1.0 + nrm(ks[6], (DEPTH, D), 0.05),
        'w_in': nrm(ks[7], (DEPTH, D, IN_WIDTH), D ** -0.5),
        'na_rpb': nrm(ks[8], (DEPTH, NA_HEADS, 2 * NA_WIN_ROWS - 1, 2 * NA_WIN_COLS - 1), 0.1),
        'hg_lb': 1.0 + nrm(ks[9], (DEPTH, 2, HG_WIDTH), 0.5),
        'hg_norm_w': 1.0 + nrm(ks[10], (DEPTH, HG_WIDTH), 0.05),
        'ml_gate_b': nrm(ks[11], (DEPTH, 2, 2, ML_HEADS), 0.1) + jnp.array([0.0, 3.0], F32)[:, None],
        'ml_norm_w': 1.0 + nrm(ks[12], (DEPTH, ML_WIDTH), 0.05),
        'w_branch': nrm(ks[13], (DEPTH, N_BRANCH, BRANCH_WIDTH, D), BRANCH_WIDTH ** -0.5),
        'w_out': nrm(ks[14], (DEPTH, D, D), D ** -0.5),
        'norm2_w': 1.0 + nrm(ks[15], (DEPTH, D), 0.05),
        'ffn_w_up': nrm(ks[16], (n_dense, D, 2 * FFN_DIM), D ** -0.5),
        'ffn_w_down': nrm(ks[17], (n_dense, FFN_DIM, D), FFN_DIM ** -0.5),
        'moe_router': nrm(ks[18], (n_moe, D, N_EXPERTS), D ** -0.5),
        'moe_w_up': nrm(ks[19], (n_moe, N_EXPERTS, D, 2 * EXPERT_DIM), D ** -0.5),
        'moe_w_down': nrm(ks[20], (n_moe, N_EXPERTS, EXPERT_DIM, D), EXPERT_DIM ** -0.5),
        'final_norm_w': 1.0 + nrm(ks[21], (D,), 0.05),
    }


def reference(x, c, ctx, c_ctx, mod_w, mod_b, norm1_w, w_in, na_rpb, hg_lb, hg_norm_w, ml_gate_b, ml_norm_w,
              w_branch, w_out, norm2_w, ffn_w_up, ffn_w_down, moe_router, moe_w_up, moe_w_down, final_norm_w):
    B, S, D = x.shape
    t = jnp.arange(S, dtype=jnp.int32)
    rows_pos, cols_pos = t // GRID_W, t % GRID_W
    lb_p = jax.nn.softmax(hg_lb.astype(F32), axis=0)
    hg_lower = jnp.cumsum(lb_p, axis=0) - lb_p[0]
    h_lat, h_ctx = x, ctx
    for layer in range(DEPTH):
        last = layer == DEPTH - 1
        mod_lat = (jax.nn.silu(c) @ mod_w[layer] + mod_b[layer]).reshape(B, 1, 6, D)
        mod_ctx = (jax.nn.silu(c_ctx) @ mod_w[layer] + mod_b[layer]).reshape(1, 1, 6, D)
        a_lat = modulate(rms_norm(h_lat, norm1_w[layer]), mod_lat[:, :, 0], mod_lat[:, :, 1])
        a_ctx = modulate(rms_norm(h_ctx, norm1_w[layer]), mod_ctx[:, :, 0], mod_ctx[:, :, 1])
        y_lat, y_ctx = hybrid_mixer(a_lat, a_ctx, w_in[layer], na_rpb[layer], hg_lower[layer], hg_norm_w[layer],
                                    ml_gate_b[layer], ml_norm_w[layer], w_branch[layer], w_out[layer],
                                    rows_pos, cols_pos, not last)
        h_lat = h_lat + mod_lat[:, :, 2] * y_lat
        if not last:
            h_ctx = h_ctx + mod_ctx[:, :, 2] * y_ctx
        f_lat = modulate(rms_norm(h_lat, norm2_w[layer]), mod_lat[:, :, 3], mod_lat[:, :, 4])
        i = layer // 2
        if layer % 2 == 0:
            h_lat = h_lat + mod_lat[:, :, 5] * swiglu(f_lat, ffn_w_up[i], ffn_w_down[i])
        else:
            h_lat = h_lat + mod_lat[:, :, 5] * moe_swiglu(f_lat, moe_router[i], moe_w_up[i], moe_w_down[i])
        if not last:
            f_ctx = modulate(rms_norm(h_ctx, norm2_w[layer]), mod_ctx[:, :, 3], mod_ctx[:, :, 4])
            if layer % 2 == 0:
                h_ctx = h_ctx + mod_ctx[:, :, 5] * swiglu(f_ctx, ffn_w_up[i], ffn_w_down[i])
            else:
                h_ctx = h_ctx + mod_ctx[:, :, 5] * moe_swiglu(f_ctx, moe_router[i], moe_w_up[i], moe_w_down[i])
    return rms_norm(h_lat, final_norm_w)
```

```python
import numpy as np
import concourse.bass as bass
import concourse.mybir as mybir
from concourse.bass_utils import run_bass_kernel_spmd
from contextlib import ExitStack

F32 = mybir.dt.float32
BF16 = mybir.dt.bfloat16
I32 = mybir.dt.int32
U32 = mybir.dt.uint32
AF = mybir.ActivationFunctionType
ALU = mybir.AluOpType
AX = mybir.AxisListType

D = 1024
KC = 8
CTX = 256
SEQ = 4096
T = CTX + SEQ
NT = T // 128
DEPTH = 2
IN_W = 9232
FFN = 2816
NEXP = 8
EDIM = 3584
EPS = 1e-6
C_NAQ, C_NAK, C_NAV = 0, 512, 1024
C_HGQ, C_HGF, C_HGB, C_HGI, C_HGG = 1536, 2048, 2560, 3072, 3584
C_MLQ, C_MLK, C_MLV, C_MLO, C_MLG, C_BG = 4096, 4608, 5120, 5632, 6144, 6160


def _interleave(*gens):
    gens = [iter(g) for g in gens]
    done = [False] * len(gens)
    while not all(done):
        for k, g in enumerate(gens):
            if not done[k]:
                try:
                    next(g)
                except StopIteration:
                    done[k] = True


class Tok:
    __slots__ = ("sem", "val", "clk")

    def __init__(self, sem, val, clk):
        self.sem, self.val, self.clk = sem, val, clk


class Buf:
    __slots__ = ("name", "w", "rs")

    def __init__(self, name):
        self.name, self.w, self.rs = name, None, {}


class Eng:
    def __init__(self, name, h, sem):
        self.name, self.h, self.sem = name, h, sem
        self.count = 0
        self.seen = {}
        self.dma_sems = []
        self.dma_vals = []
        self.dma_rr = 0


class Sched:
    def __init__(self, nc, stack, n_dma_sems=12):
        self.nc = nc
        mk = lambda n: stack.enter_context(nc.semaphore(n))
        self.pe = Eng("pe", nc.tensor, mk("s_pe"))
        self.act = Eng("act", nc.scalar, mk("s_act"))
        self.dve = Eng("dve", nc.vector, mk("s_dve"))
        self.pool = Eng("pool", nc.gpsimd, mk("s_pool"))
        self.sp = Eng("sp", nc.sync, mk("s_sp"))
        self.engs = [self.pe, self.act, self.dve, self.pool, self.sp]
        for q in (self.sp, self.pool):
            for i in range(n_dma_sems):
                q.dma_sems.append(mk(f"d_{q.name}{i}"))
                q.dma_vals.append(0)
        self.n_inst = 0

    def _need(self, eng, tok, kind):
        if tok is None:
            return
        if tok.sem is eng.sem:
            if eng is self.pe:
                return
        k = id(tok.sem)
        if eng.seen.get(k, 0) >= tok.val:
            return
        eng.h.wait_ge(tok.sem, tok.val)
        self.n_inst += 1
        seen = eng.seen
        for kk, vv in tok.clk.items():
            if seen.get(kk, 0) < vv:
                seen[kk] = vv
        seen[k] = tok.val

    def _deps(self, eng, reads, writes):
        for b in reads:
            self._need(eng, b.w, "raw")
        for b in writes:
            self._need(eng, b.w, "waw")
            for r in b.rs.values():
                self._need(eng, r, "war")

    def _commit(self, tok, reads, writes):
        k = id(tok.sem)
        for b in reads:
            o = b.rs.get(k)
            if o is None or o.val < tok.val:
                b.rs[k] = tok
        for b in writes:
            b.w = tok
            b.rs = {}

    def op(self, eng, fn, reads=(), writes=(), inc=True):
        self._deps(eng, reads, writes)
        ins = fn(eng.h)
        self.n_inst += 1
        if inc:
            ins.then_inc(eng.sem, 1)
            eng.count += 1
            val = eng.count
        else:
            val = eng.count + 1
        clk = dict(eng.seen)
        tok = Tok(eng.sem, val, clk)
        self._commit(tok, reads, writes)
        return tok

    def dma(self, q, fn, reads=(), writes=()):
        i = q.dma_rr
        q.dma_rr = (i + 1) % len(q.dma_sems)
        sem = q.dma_sems[i]
        prev = q.dma_vals[i]
        k = id(sem)
        if prev and q.seen.get(k, 0) < prev:
            q.h.wait_ge(sem, prev)
            q.seen[k] = prev
        self._deps(q, reads, writes)
        ins = fn(q.h)
        ins.then_inc(sem, 16)
        self.n_inst += 1
        q.dma_vals[i] = prev + 16
        tok = Tok(sem, prev + 16, dict(q.seen))
        self._commit(tok, reads, writes)
        return tok

    def barrier(self):
        for e in self.engs:
            for f in self.engs:
                if f is not e and f.count and e.seen.get(id(f.sem), 0) < f.count:
                    e.h.wait_ge(f.sem, f.count)
                    e.seen[id(f.sem)] = f.count
            for q in (self.sp, self.pool):
                for sem, v in zip(q.dma_sems, q.dma_vals):
                    if v and e.seen.get(id(sem), 0) < v:
                        e.h.wait_ge(sem, v)
                        e.seen[id(sem)] = v


class Prog:
    def __init__(self, upto="all", debug=()):
        self.upto = upto
        self.debug = set(debug)
        self.nc = nc = bass.Bass("TRN2", target_bir_lowering=False)
        self.stack = ExitStack()
        self.S = Sched(nc, self.stack)
        self.dram = {}
        self.bufs = {}

    def din(self, name, shape, dt=F32):
        self.in_names = getattr(self, "in_names", set()) | {name}
        t = self.nc.dram_tensor(name, list(shape), dt, kind="ExternalInput").ap()
        self.dram[name] = t
        self.bufs[name] = Buf(name)
        return t

    def dout(self, name, shape, dt=F32):
        t = self.nc.dram_tensor(name, list(shape), dt, kind="ExternalOutput").ap()
        self.dram[name] = t
        self.bufs[name] = Buf(name)
        return t

    def dscr(self, name, shape, dt=F32):
        t = self.nc.dram_tensor(name, list(shape), dt).ap()
        self.dram[name] = t
        self.bufs[name] = Buf(name)
        return t

    def sb(self, ctx, name, shape, dt):
        self.uid = getattr(self, "uid", 0) + 1
        return ctx.enter_context(self.nc.sbuf_tensor(f"{name}_u{self.uid}", list(shape), dt))

    def declare_io(self):
        self.din("xin", [T, D])
        self.din("cvec", [128, KC, 2])
        self.din("ident", [128, 128])
        self.din("mod_w", [DEPTH, D, 6 * D])
        self.din("mod_b", [DEPTH, 6 * D])
        self.din("norm1_w", [DEPTH, D])
        self.din("norm2_w", [DEPTH, D])
        self.din("w_in", [DEPTH, D, IN_W])
        self.dscr("modv", [DEPTH, 2, 8, D])
        self.dscr("hbuf", [T, D])

    def stage_mod(self, layer):
        nc, S = self.nc, self.S
        B = self.bufs
        with ExitStack() as c:
            cv = self.sb(c, "m_cv", [128, KC, 2], F32)
            cs = self.sb(c, "m_cs", [128, KC, 2], BF16)
            sg = self.sb(c, "m_sg", [128, KC, 2], F32)
            wt = [self.sb(c, f"m_wt{i}", [128, KC, 512], BF16) for i in range(2)]
            res = self.sb(c, "m_res", [2, 8, D], F32)
            mb = self.sb(c, "m_mb", [2, 6 * D], F32)
            nw = self.sb(c, "m_nw", [2, 2, D], F32)
            b_cv, b_cs, b_sg, b_res, b_mb, b_nw = (Buf(n) for n in ("cv", "cs", "sg", "res", "mb", "nw"))
            b_wt = [Buf("wt0"), Buf("wt1")]
            ps = self.ps
            S.dma(S.sp, lambda e: e.dma_start(out=cv[:], in_=self.dram["cvec"][:, :, :]), [B["cvec"]], [b_cv])
            for r in range(2):
                S.dma(S.sp, lambda e, r=r: e.dma_start(out=mb[r:r + 1, :], in_=self.dram["mod_b"][layer:layer + 1, :]),
                      [B["mod_b"]], [b_mb])
                S.dma(S.sp, lambda e, r=r: e.dma_start(out=nw[r:r + 1, 0, :], in_=self.dram["norm1_w"][layer:layer + 1, :]),
                      [B["norm1_w"]], [b_nw])
                S.dma(S.sp, lambda e, r=r: e.dma_start(out=nw[r:r + 1, 1, :], in_=self.dram["norm2_w"][layer:layer + 1, :]),
                      [B["norm2_w"]], [b_nw])
            S.op(S.act, lambda e: e.activation(out=sg[:], in_=cv[:], func=AF.Sigmoid), [b_cv], [b_sg])
            S.op(S.dve, lambda e: e.tensor_tensor(out=cs[:], in0=cv[:], in1=sg[:], op=ALU.mult), [b_cv, b_sg], [b_cs])
            mw = self.dram["mod_w"]
            for j in range(12):
                w = wt[j % 2]
                S.dma(S.pool, lambda e, j=j, w=w: e.dma_start(
                    out=w[:], in_=mw[layer, :, j * 512:(j + 1) * 512].rearrange("(kc p) n -> p kc n", p=128)),
                    [B["mod_w"]], [b_wt[j % 2]])
                pb = self.psb[j % 2]
                for k in range(KC):
                    S.op(S.pe, lambda e, k=k, w=w, j=j: e.matmul(ps[j % 2][0:2, :], lhsT=cs[:, k, :], rhs=w[:, k, :],
                                                              start=(k == 0), stop=(k == KC - 1)),
                         [b_cs, b_wt[j % 2]], [pb], inc=(k == KC - 1))
                g, o = divmod(j * 512, D)
                S.op(S.dve, lambda e, j=j, g=g, o=o: e.tensor_tensor(
                    out=res[:, g, o:o + 512], in0=ps[j % 2][0:2, :], in1=mb[:, j * 512:(j + 1) * 512], op=ALU.add),
                    [pb, b_mb], [b_res])
            for r, (sc, wi) in enumerate(((1, 0), (4, 1))):
                S.op(S.dve, lambda e, r=r, sc=sc, wi=wi: e.scalar_tensor_tensor(
                    out=res[:, 6 + r, :], in0=res[:, sc, :], scalar=1.0, in1=nw[:, wi, :], op0=ALU.add, op1=ALU.mult),
                    [b_res, b_nw], [b_res])
            S.dma(S.sp, lambda e: e.dma_start(out=self.dram["modv"][layer], in_=res[:]), [b_res], [B["modv"]])
            S.barrier()

    def load_row_bcast(self, dst, layer, r, row, dst_buf):
        src = self.dram["modv"][layer, r, row:row + 1, :].partition_broadcast(128)
        self.S.dma(self.S.sp, lambda e: e.dma_start(out=dst, in_=src), [self.bufs["modv"]], [dst_buf])

    def stage_norm(self, layer, which, src_name, aT, aT_bufs, tiles, router=None, gather=None):
        nc, S, B = self.nc, self.S, self.bufs
        grow, srow = ((6, 0), (7, 3))[which]
        src = self.dram[src_name]
        with ExitStack() as c:
            gv = [self.sb(c, f"n_gv{r}", [128, D], F32) for r in range(2)]
            sv = [self.sb(c, f"n_sv{r}", [128, D], F32) for r in range(2)]
            b_gv = [Buf("gv0"), Buf("gv1")]
            b_sv = [Buf("sv0"), Buf("sv1")]
            idb = self.sb(c, "n_id", [128, 128], BF16)
            b_id = Buf("id")
            S.dma(S.pool, lambda e: e.dma_start(out=idb[:], in_=self.dram["ident"][:, :]), [B["ident"]], [b_id])
            for r in range(2):
                self.load_row_bcast(gv[r][:], layer, r, grow, b_gv[r])
                self.load_row_bcast(sv[r][:], layer, r, srow, b_sv[r])
            NB = 3
            ht = [self.sb(c, f"n_h{i}", [128, D], F32) for i in range(NB)]
            b_h = [Buf(f"h{i}") for i in range(NB)]
            sq = self.sb(c, "n_sq", [128, D], BF16)
            b_sq = Buf("sq")
            st = [self.sb(c, f"n_st{i}", [128, 4], F32) for i in range(NB)]
            b_st = [Buf(f"st{i}") for i in range(NB)]
            t1 = [self.sb(c, f"n_t1{i}", [128, D], F32) for i in range(2)]
            b_t1 = [Buf("t10"), Buf("t11")]
            ab = [self.sb(c, f"n_ab{i}", [128, D], BF16) for i in range(2)]
            b_ab = [Buf("ab0"), Buf("ab1")]

            def load(i):
                ti = tiles[i]
                if gather is None:
                    S.dma(S.sp, lambda e: e.dma_start(out=ht[i % NB][:], in_=src[ti * 128:(ti + 1) * 128, :]),
                          [B[src_name]] + ([self.h_bufs[ti]] if src_name == "hbuf" else []), [b_h[i % NB]])
                else:
                    S.dma(S.pool, lambda e: e.indirect_dma_start(out=ht[i % NB][:], out_offset=None, in_=src[:, :],
                                                                 in_offset=bass.IndirectOffsetOnAxis(ap=gather[0][:, ti - 2:ti - 1], axis=0)),
                          [B[src_name], gather[1]] + self.h_bufs, [b_h[i % NB]])

            def statsA(i, ti):
                    r = 1 if ti < CTX // 128 else 0
                    h, s_, bh, bs = ht[i % NB], st[i % NB], b_h[i % NB], b_st[i % NB]
                    S.op(S.act, lambda e: e.activation(out=sq[:], in_=h[:], func=AF.Square, accum_out=s_[:, 0:1]),
                         [bh], [b_sq, bs])
                    yield
                    S.op(S.dve, lambda e: e.tensor_scalar(out=s_[:, 1:2], in0=s_[:, 0:1], scalar1=1.0 / D, scalar2=EPS,
                                                          op0=ALU.mult, op1=ALU.add), [bs], [bs])
                    yield
                    S.op(S.act, lambda e: e.activation(out=s_[:, 2:3], in_=s_[:, 1:2], func=AF.Ln), [bs], [bs])
                    yield
                    S.op(S.act, lambda e: e.activation(out=s_[:, 3:4], in_=s_[:, 2:3], func=AF.Exp, scale=-0.5), [bs], [bs])
                    yield

            def applyB(i, ti):
                    r = 1 if ti < CTX // 128 else 0
                    h, s_, bh, bs = ht[i % NB], st[i % NB], b_h[i % NB], b_st[i % NB]
                    tt, bt = t1[i % 2], b_t1[i % 2]
                    a_, ba = ab[i % 2], b_ab[i % 2]
                    S.op(S.dve, lambda e: e.scalar_tensor_tensor(out=tt[:], in0=h[:], scalar=s_[:, 3:4], in1=gv[r][:],
                                                                 op0=ALU.mult, op1=ALU.mult), [bh, bs, b_gv[r]], [bt])
                    yield
                    if router is None:
                        S.op(S.pool, lambda e: e.tensor_tensor(out=a_[:], in0=tt[:], in1=sv[r][:], op=ALU.add),
                             [bt, b_sv[r]], [ba])
                        yield
                    else:
                        gates, b_gates, wr, b_wr, rs, b_rs, junk, b_junk = router
                        S.op(S.pool, lambda e: e.tensor_tensor(out=tt[:], in0=tt[:], in1=sv[r][:], op=ALU.add), [bt, b_sv[r]], [bt])
                        yield
                        S.op(S.act, lambda e: e.copy(out=a_[:], in_=tt[:]), [bt], [ba])
                        yield
                        for ex in range(NEXP):
                            S.op(S.dve, lambda e: e.scalar_tensor_tensor(out=junk[:], in0=tt[:], scalar=1.0, in1=wr[:, ex, :], op0=ALU.mult, op1=ALU.mult,
                                                                         accum_out=rs[:, ex:ex + 1]), [bt, b_wr[ex]], [b_junk, b_rs])
                            yield
                        lg, mk1, lg2, mk2 = rs[:, 0:8], rs[:, 8:16], rs[:, 16:24], rs[:, 24:32]
                        S.op(S.dve, lambda e: e.tensor_reduce(out=rs[:, 32:33], in_=lg, op=ALU.max, axis=AX.X), [b_rs], [b_rs])
                        yield
                        S.op(S.dve, lambda e: e.tensor_scalar(out=mk1, in0=lg, scalar1=rs[:, 32:33], scalar2=None, op0=ALU.is_equal), [b_rs], [b_rs])
                        yield
                        S.op(S.dve, lambda e: e.scalar_tensor_tensor(out=lg2, in0=mk1, scalar=-1e30, in1=lg, op0=ALU.mult, op1=ALU.add), [b_rs], [b_rs])
                        yield
                        S.op(S.dve, lambda e: e.tensor_reduce(out=rs[:, 33:34], in_=lg2, op=ALU.max, axis=AX.X), [b_rs], [b_rs])
                        yield
                        S.op(S.dve, lambda e: e.tensor_scalar(out=mk2, in0=lg2, scalar1=rs[:, 33:34], scalar2=None, op0=ALU.is_equal), [b_rs], [b_rs])
                        yield
                        S.op(S.dve, lambda e: e.tensor_tensor(out=rs[:, 34:35], in0=rs[:, 33:34], in1=rs[:, 32:33], op=ALU.subtract), [b_rs], [b_rs])
                        yield
                        S.op(S.act, lambda e: e.activation(out=rs[:, 35:36], in_=rs[:, 34:35], func=AF.Exp), [b_rs], [b_rs])
                        yield
                        S.op(S.dve, lambda e: e.tensor_scalar(out=rs[:, 36:37], in0=rs[:, 35:36], scalar1=1.0, scalar2=None, op0=ALU.add), [b_rs], [b_rs])
                        yield
                        S.op(S.dve, lambda e: e.reciprocal(out=rs[:, 37:38], in_=rs[:, 36:37]), [b_rs], [b_rs])
                        yield
                        S.op(S.dve, lambda e: e.tensor_tensor(out=rs[:, 38:39], in0=rs[:, 35:36], in1=rs[:, 37:38], op=ALU.mult), [b_rs], [b_rs])
                        yield
                        S.op(S.dve, lambda e: e.tensor_scalar(out=rs[:, 40:48], in0=mk1, scalar1=rs[:, 37:38], scalar2=None, op0=ALU.mult), [b_rs], [b_rs])
                        yield
                        S.op(S.dve, lambda e: e.scalar_tensor_tensor(out=gates[:, ti, :], in0=mk2, scalar=rs[:, 38:39], in1=rs[:, 40:48], op0=ALU.mult, op1=ALU.add),
                             [b_rs], [b_gates])
                        yield
                    pb = self.psb[i % 2]
                    pt = self.ps[i % 2][:].bitcast(BF16)
                    for k in range(KC):
                        S.op(S.pe, lambda e, k=k: e.transpose(pt[:, k * 128:(k + 1) * 128], a_[:, k * 128:(k + 1) * 128], idb[:]),
                             [ba, b_id], [pb], inc=(k == KC - 1))
                        yield
                    eng = S.act if i % 2 == 0 else S.dve
                    dst = aT[:, :, ti * 128:(ti + 1) * 128]
                    srcp = pt.rearrange("p (k t) -> p k t", k=KC)
                    if eng is S.act:
                        S.op(eng, lambda e: e.copy(out=dst, in_=srcp), [pb], [aT_bufs[ti]])
                        yield
                    else:
                        S.op(eng, lambda e: e.tensor_copy(out=dst, in_=srcp), [pb], [aT_bufs[ti]])
                        yield

            for i in range(min(2, len(tiles))):
                load(i)
            for _ in statsA(0, tiles[0]):
                pass
            for i, ti in enumerate(tiles):
                nxt = statsA(i + 1, tiles[i + 1]) if i + 1 < len(tiles) else iter(())
                _interleave(nxt, applyB(i, ti))
                if i + 2 < len(tiles):
                    load(i + 2)
            S.barrier()


    def declare_proj(self):
        self.dscr("p_naqk", [1024, T], BF16)
        for n in ("p_nav", "p_hgq", "p_hgi", "p_hgg", "p_mlq", "p_mlk", "p_mlv", "p_mlo"):
            self.dscr(n, [T, 512], BF16)
        self.dscr("p_hgf", [T, 512], F32)
        self.dscr("p_hgb", [T, 512], F32)
        self.dscr("p_mlg", [T, 16], F32)

    def stage_inproj(self, layer, aT, aT_bufs):
        nc, S, B = self.nc, self.S, self.bufs
        w_in = self.dram["w_in"]
        tm_blocks = [("p_nav", C_NAV, BF16), ("p_hgq", C_HGQ, BF16), ("p_hgf", C_HGF, F32), ("p_hgb", C_HGB, F32),
                     ("p_hgi", C_HGI, BF16), ("p_hgg", C_HGG, BF16), ("p_mlq", C_MLQ, BF16), ("p_mlk", C_MLK, BF16),
                     ("p_mlv", C_MLV, BF16), ("p_mlo", C_MLO, BF16)]
        with ExitStack() as c:
            wt = [self.sb(c, f"p_wt{i}", [128, KC, 512], BF16) for i in range(2)]
            b_wt = [Buf("pwt0"), Buf("pwt1")]
            wg = self.sb(c, "p_wg", [128, KC, 16], BF16)
            b_wg = Buf("pwg")
            stg_b = [self.sb(c, f"p_sb{i}", [128, 4, 512], BF16) for i in range(2)]
            stg_f = [self.sb(c, f"p_sf{i}", [128, 4, 512], F32) for i in range(2)]
            b_sb = [Buf("psb0"), Buf("psb1")]
            b_sf = [Buf("psf0"), Buf("psf1")]
            stg_g = self.sb(c, "p_sg", [128, NT, 16], F32)
            b_sg = Buf("psg")
            cnt = {"w": 0, "ev": 0, "ps": 0, "st": 0}

            def load_w(col0, width=512):
                i = cnt["w"] % 2
                cnt["w"] += 1
                S.dma(S.pool, lambda e: e.dma_start(
                    out=wt[i][:, :, 0:width],
                    in_=w_in[layer, :, col0:col0 + width].rearrange("(kc p) n -> p kc n", p=128)),
                    [B["w_in"]], [b_wt[i]])
                return wt[i], b_wt[i]

            def evac(dst, src, rd, wr):
                if cnt["ev"] % 2 == 0:
                    S.op(S.act, lambda e: e.copy(out=dst, in_=src), rd, wr)
                else:
                    S.op(S.dve, lambda e: e.tensor_copy(out=dst, in_=src), rd, wr)
                cnt["ev"] += 1

            def next_ps():
                i = cnt["ps"] % 4
                cnt["ps"] += 1
                return self.ps[i], self.psb[i]

            blocks = [("fm", "q", C_NAQ), ("fm", "k", C_NAK)] + [("tm",) + b for b in tm_blocks]
            pending = load_w(blocks[0][2])
            for bi, blk in enumerate(blocks):
                w, bw = pending
                if bi + 1 < len(blocks):
                    nb = blocks[bi + 1]
                    pending = load_w(nb[2])
                if blk[0] == "fm":
                    row0 = 0 if blk[1] == "q" else 512
                    for m in range(4):
                        for tb in range(0, T, 512):
                            n = min(512, T - tb)
                            ps, pb = next_ps()
                            tiles = range(tb // 128, (tb + n) // 128)
                            for k in range(KC):
                                S.op(S.pe, lambda e: e.matmul(ps[:, 0:n], lhsT=w[:, k, m * 128:(m + 1) * 128],
                                                              rhs=aT[:, k, tb:tb + n], start=(k == 0), stop=(k == KC - 1)),
                                     [bw] + [aT_bufs[t] for t in tiles], [pb], inc=(k == KC - 1))
                            si = cnt["st"] % 2
                            cnt["st"] += 1
                            evac(stg_b[si][:, 0, 0:n], ps[:, 0:n], [pb], [b_sb[si]])
                            S.dma(S.sp, lambda e: e.dma_start(
                                out=self.dram["p_naqk"][row0 + m * 128:row0 + (m + 1) * 128, tb:tb + n],
                                in_=stg_b[si][:, 0, 0:n]), [b_sb[si]], [B["p_naqk"]])
                else:
                    _, name, col0, dt = blk
                    stg, bst = (stg_b, b_sb) if dt == BF16 else (stg_f, b_sf)
                    for g0 in range(0, NT, 4):
                        gn = min(4, NT - g0)
                        si = cnt["st"] % 2
                        cnt["st"] += 1
                        for j in range(gn):
                            ti = g0 + j
                            ps, pb = next_ps()
                            for k in range(KC):
                                S.op(S.pe, lambda e: e.matmul(ps[:, :], lhsT=aT[:, k, ti * 128:(ti + 1) * 128],
                                                              rhs=w[:, k, :], start=(k == 0), stop=(k == KC - 1)),
                                     [bw, aT_bufs[ti]], [pb], inc=(k == KC - 1))
                            evac(stg[si][:, j, :], ps[:, :], [pb], [bst[si]])
                        S.dma(S.sp, lambda e: e.dma_start(
                            out=self.dram[name][g0 * 128:(g0 + gn) * 128, :].rearrange("(j p) n -> p j n", p=128),
                            in_=stg[si][:, 0:gn, :]), [bst[si]], [B[name]])
            S.dma(S.pool, lambda e: e.dma_start(
                out=wg[:], in_=w_in[layer, :, C_MLG:C_MLG + 16].rearrange("(kc p) n -> p kc n", p=128)),
                [B["w_in"]], [b_wg])
            for ti in range(NT):
                ps, pb = next_ps()
                for k in range(KC):
                    S.op(S.pe, lambda e: e.matmul(ps[:, 0:16], lhsT=aT[:, k, ti * 128:(ti + 1) * 128],
                                                  rhs=wg[:, k, :], start=(k == 0), stop=(k == KC - 1)),
                         [b_wg, aT_bufs[ti]], [pb], inc=(k == KC - 1))
                evac(stg_g[:, ti, :], ps[:, 0:16], [pb], [b_sg])
            S.dma(S.sp, lambda e: e.dma_start(out=self.dram["p_mlg"].rearrange("(j p) n -> p j n", p=128), in_=stg_g[:]),
                  [b_sg], [B["p_mlg"]])
            S.barrier()


    def TB(self, ctx, name, shape, dt):
        return self.sb(ctx, name, shape, dt), Buf(name)

    def declare_scan(self):
        self.din("mx", [2, 128, 128])
        self.din("sel", [2, 128, 6])
        self.din("selrep", [2, 6, 128, 128])
        self.din("amask", [2, 128, 4, 128])
        self.din("rope", [SEQ, 4, 32])
        self.din("hg_lb", [DEPTH, 2, 512])
        self.din("hg_norm_w", [DEPTH, 512])
        self.din("ml_norm_w", [DEPTH, 512])
        self.din("ml_gate_b", [DEPTH, 16])
        self.dscr("of_hg", [T, 512], F32)
        self.dscr("of_ml", [T, 512], F32)
        self.dscr("brT", [3, 512, T], BF16)

    def stage_scan(self, layer, kind, last):
        nc, S, B, ps, psb = self.nc, self.S, self.bufs, self.ps, self.psb
        H = 4
        DV = 128 if kind == "hg" else 129
        dr = self.dram
        of_name = "of_hg" if kind == "hg" else "of_ml"
        br_idx = 1 if kind == "hg" else 2
        with ExitStack() as c:
            idb, b_id = self.TB(c, "s_id", [128, 128], BF16)
            S.dma(S.pool, lambda e: e.dma_start(out=idb[:], in_=dr["ident"][:, :]), [B["ident"]], [b_id])
            mx, b_mx = self.TB(c, "s_mx", [128, 2, 128], F32)
            S.dma(S.sp, lambda e: e.dma_start(out=mx[:], in_=dr["mx"].rearrange("d s t -> s d t")), [B["mx"]], [b_mx])
            am, b_am = self.TB(c, "s_am", [128, 2, 4 * 128], F32)
            S.dma(S.sp, lambda e: e.dma_start(out=am[:], in_=dr["amask"].rearrange("d p h t -> p d (h t)")), [B["amask"]], [b_am])
            nwn = "hg_norm_w" if kind == "hg" else "ml_norm_w"
            nw, b_nw = self.TB(c, "s_nw", [128, 512], F32)
            S.dma(S.sp, lambda e: e.dma_start(out=nw[:], in_=dr[nwn][layer:layer + 1, :].partition_broadcast(128)), [B[nwn]], [b_nw])
            if kind == "hg":
                selt, b_sel = self.TB(c, "s_sel", [128, 2, 6], F32)
                S.dma(S.sp, lambda e: e.dma_start(out=selt[:], in_=dr["sel"].rearrange("d s j -> s d j")), [B["sel"]], [b_sel])
                lb, b_lb = self.TB(c, "s_lb", [128, 2, 512], F32)
                oml, b_oml = self.TB(c, "s_oml", [128, 2, 512], F32)
                if layer == 0:
                    S.op(S.dve, lambda e: e.memset(lb[:], 0.0), [], [b_lb])
                    S.op(S.dve, lambda e: e.memset(oml[:], 1.0), [], [b_oml])
                else:
                    l0, b_l0 = self.TB(c, "s_l0", [128, 2, 512], F32)
                    l1, b_l1 = self.TB(c, "s_l1", [128, 2, 512], F32)
                    for dd in range(2):
                        S.dma(S.sp, lambda e: e.dma_start(out=l0[:, dd, :], in_=dr["hg_lb"][0, dd:dd + 1, :].partition_broadcast(128)), [B["hg_lb"]], [b_l0])
                        S.dma(S.sp, lambda e: e.dma_start(out=l1[:, dd, :], in_=dr["hg_lb"][1, dd:dd + 1, :].partition_broadcast(128)), [B["hg_lb"]], [b_l1])
                    S.op(S.dve, lambda e: e.tensor_tensor(out=l0[:], in0=l0[:], in1=l1[:], op=ALU.subtract), [b_l0, b_l1], [b_l0])
                    S.op(S.act, lambda e: e.activation(out=l1[:], in_=l0[:], func=AF.Exp), [b_l0], [b_l1])
                    S.op(S.dve, lambda e: e.tensor_scalar(out=l0[:], in0=l1[:], scalar1=1.0, scalar2=None, op0=ALU.add), [b_l1], [b_l0])
                    S.op(S.dve, lambda e: e.reciprocal(out=lb[:], in_=l0[:]), [b_l0], [b_lb])
                    S.op(S.dve, lambda e: e.tensor_tensor(out=oml[:], in0=l1[:], in1=lb[:], op=ALU.mult), [b_l1, b_lb], [b_oml])
            else:
                selr, b_selr = self.TB(c, "s_selr", [128, 2, 6, 128], F32)
                S.dma(S.sp, lambda e: e.dma_start(out=selr[:], in_=dr["selrep"].rearrange("d j s m -> s d j m")), [B["selrep"]], [b_selr])
                gb, b_gb = self.TB(c, "s_gb", [128, 16], F32)
                S.dma(S.sp, lambda e: e.dma_start(out=gb[:], in_=dr["ml_gate_b"][layer:layer + 1, :].partition_broadcast(128)), [B["ml_gate_b"]], [b_gb])
            NB = 3
            inq = [self.TB(c, f"s_inq{i}", [128, 512], BF16) for i in range(NB)]
            inv = [self.TB(c, f"s_inv{i}", [128, 512], BF16) for i in range(NB)]
            if kind == "hg":
                inz = [self.TB(c, f"s_inz{i}", [128, 512], F32) for i in range(NB)]
            else:
                ink = [self.TB(c, f"s_ink{i}", [128, 512], BF16) for i in range(NB)]
                ing = [self.TB(c, f"s_ing{i}", [128, 16], F32) for i in range(NB)]
                inr = [self.TB(c, f"s_inr{i}", [128, 4, 32], F32) for i in range(NB)]
            ing2 = [self.TB(c, f"s_ing2{i}", [128, 512], BF16) for i in range(NB)]
            inof = [self.TB(c, f"s_inof{i}", [128, 512], F32) for i in range(NB)]
            f1, b_f1 = self.TB(c, "s_f1", [128, 512], F32)
            f2, b_f2 = self.TB(c, "s_f2", [128, 512], F32)
            f3, b_f3 = self.TB(c, "s_f3", [128, 512], F32)
            f4, b_f4 = self.TB(c, "s_f4", [128, 512], F32)
            f5, b_f5 = self.TB(c, "s_f5", [128, 512], F32)
            lf, b_lf = self.TB(c, "s_lf", [128, 512], F32)
            sm, b_sm = self.TB(c, "s_sm", [128, 64], F32)
            sc_, b_sc = self.TB(c, "s_sc", [128, 64], F32)
            qt = [self.TB(c, f"s_qt{i}", [128, 512], BF16) for i in range(NB)]
            kk = [self.TB(c, f"s_kk{i}", [128, 512], BF16) for i in range(NB)]
            vv = [self.TB(c, f"s_vv{i}", [128, H, DV], BF16) for i in range(NB)]
            qkT = [self.TB(c, f"s_qkT{i}", [128, 8, 128], BF16) for i in range(NB)]
            fac = [self.TB(c, f"s_fac{i}", [128, H, 6], F32) for i in range(NB)]
            Am, b_Am = self.TB(c, "s_Am", [128, H * 128], BF16)
            St, b_St = self.TB(c, "s_St", [128, H, DV], F32)
            Sp, b_Sp = self.TB(c, "s_Sp", [128, H, DV], BF16)
            kvt, b_kvt = self.TB(c, "s_kvt", [128, H, DV], F32)
            osbs = [self.TB(c, f"s_osb{i}", [128, 512], F32) for i in range(2)]
            sr, b_sr = self.TB(c, "s_sr", [128, 64], F32)
            ro1, b_ro1 = self.TB(c, "s_ro1", [128, 512], F32)
            ro2, b_ro2 = self.TB(c, "s_ro2", [128, 512], F32)
            rob, b_rob = self.TB(c, "s_rob", [128, 512], BF16)
            brs, b_brs = self.TB(c, "s_brs", [128, 4, 128], BF16)
            if kind == "ml":
                for i in range(NB):
                    S.op(S.dve, lambda e: e.memset(vv[i][0][:, :, 128:129], 1.0), [], [vv[i][1]])
            pX, bX = ps[0], psb[0]
            pT, bT = ps[1][:].bitcast(BF16), psb[1]
            pA, bA = ps[2], psb[2]
            pO = [ps[3], ps[4]]; bO = [psb[3], psb[4]]
            pK = [ps[5], ps[6]]; bK = [psb[5], psb[6]]
            pR, bR = ps[7], psb[7]
            pF, bF = ps[7], Buf("psF")
            pOv = [p[:, 0:2 * DV].rearrange("p (h d) -> p h d", h=2) for p in pO]
            pKv = [p[:, 0:2 * DV].rearrange("p (h d) -> p h d", h=2) for p in pK]
            names = {"hg": ("p_hgq", ("p_hgf", "p_hgb"), "p_hgi", "p_hgg"), "ml": ("p_mlq", "p_mlk", "p_mlv", "p_mlo")}[kind]

            def exp_(out, in_, rd, wr, scale=1.0, bias=None):
                if bias is None:
                    S.op(S.act, lambda e: e.activation(out=out, in_=in_, func=AF.Exp, scale=scale), rd, wr)
                    yield
                else:
                    S.op(S.act, lambda e: e.activation(out=out, in_=in_, func=AF.Exp, scale=scale, bias=bias), rd, wr)
                    yield

            def load(slot, ti, d):
                rows = slice(ti * 128, (ti + 1) * 128)
                S.dma(S.sp, lambda e: e.dma_start(out=inq[slot][0][:], in_=dr[names[0]][rows, :]), [B[names[0]]], [inq[slot][1]])
                S.dma(S.sp, lambda e: e.dma_start(out=inv[slot][0][:], in_=dr[names[2]][rows, :]), [B[names[2]]], [inv[slot][1]])
                if kind == "hg":
                    zn = names[1][d]
                    S.dma(S.sp, lambda e: e.dma_start(out=inz[slot][0][:], in_=dr[zn][rows, :]), [B[zn]], [inz[slot][1]])
                else:
                    S.dma(S.sp, lambda e: e.dma_start(out=ink[slot][0][:], in_=dr[names[1]][rows, :]), [B[names[1]]], [ink[slot][1]])
                    S.dma(S.sp, lambda e: e.dma_start(out=ing[slot][0][:], in_=dr["p_mlg"][rows, :]), [B["p_mlg"]], [ing[slot][1]])
                    if ti >= 2:
                        S.dma(S.sp, lambda e: e.dma_start(out=inr[slot][0][:], in_=dr["rope"][(ti - 2) * 128:(ti - 1) * 128, :, :]),
                              [B["rope"]], [inr[slot][1]])
                if d == 1 and not (last and ti < 2):
                    S.dma(S.sp, lambda e: e.dma_start(out=ing2[slot][0][:], in_=dr[names[3]][rows, :]), [B[names[3]]], [ing2[slot][1]])
                    S.dma(S.sp, lambda e: e.dma_start(out=inof[slot][0][:], in_=dr[of_name][rows, :]), [B[of_name]], [inof[slot][1]])

            def prep_hg(slot, ti, d):
                q, bq = inq[slot]; z, bz = inz[slot]; v, bv = inv[slot]
                yield from exp_(f1[:], z[:], [bz], [b_f1], scale=-1.0)
                S.op(S.act, lambda e: e.activation(out=f1[:], in_=f1[:], func=AF.Ln, bias=1.0), [b_f1], [b_f1])
                yield
                yield from exp_(f2[:], f1[:], [b_f1], [b_f2], scale=-1.0)
                S.op(S.dve, lambda e: e.tensor_tensor(out=f2[:], in0=f2[:], in1=oml[:, d, :], op=ALU.mult), [b_f2, b_oml], [b_f2])
                yield
                S.op(S.dve, lambda e: e.scalar_tensor_tensor(out=f3[:], in0=f2[:], scalar=1e-30, in1=lb[:, d, :], op0=ALU.max, op1=ALU.add),
                     [b_f2, b_lb], [b_f3])
                yield
                S.op(S.act, lambda e: e.activation(out=lf[:], in_=f3[:], func=AF.Ln), [b_f3], [b_lf])
                yield
                S.op(S.pool, lambda e: e.tensor_tensor(out=f3[:], in0=oml[:, d, :], in1=f2[:], op=ALU.subtract), [b_oml, b_f2, b_lf], [b_f3])
                yield
                S.op(S.pe, lambda e: e.matmul(pX[:, :], lhsT=mx[:, d, :], rhs=lf[:], start=True, stop=True), [b_mx, b_lf], [bX])
                yield
                for h in range(H):
                    S.op(S.pe, lambda e: e.matmul(pF[:, 256 + h * 6:256 + h * 6 + 6], lhsT=lf[:, h * 128:(h + 1) * 128], rhs=selt[:, d, :],
                                                  start=True, stop=True), [b_lf, b_sel], [bF], inc=(h == H - 1))
                    yield
                fc, bfc = fac[slot]
                yield from exp_(fc[:].rearrange("p h j -> p (h j)"), pF[:, 256:256 + H * 6], [bF], [bfc])
                yield from exp_(f1[:], q[:], [bq], [b_f1], scale=-1.0)
                S.op(S.act, lambda e: e.activation(out=f1[:], in_=f1[:], func=AF.Ln, bias=1.0), [b_f1], [b_f1])
                yield
                yield from exp_(f2[:], f1[:], [b_f1], [b_f2], scale=-1.0)
                S.op(S.pool, lambda e: e.tensor_tensor(out=f2[:], in0=f2[:], in1=q[:], op=ALU.mult), [b_f2, bq], [b_f2])
                yield
                yield from exp_(f4[:], pX[:, :], [bX], [b_f4])
                yield from exp_(f5[:], pX[:, :], [bX], [b_f5], scale=-1.0)
                S.op(S.dve, lambda e: e.tensor_tensor(out=qt[slot][0][:], in0=f2[:], in1=f4[:], op=ALU.mult), [b_f2, b_f4], [qt[slot][1]])
                yield
                S.op(S.dve, lambda e: e.tensor_tensor(out=kk[slot][0][:], in0=f3[:], in1=f5[:], op=ALU.mult), [b_f3, b_f5], [kk[slot][1]])
                yield
                S.op(S.pool, lambda e: e.tensor_copy(out=vv[slot][0][:].rearrange("p h d -> p (h d)"), in_=v[:]), [bv], [vv[slot][1]])
                yield

            def rope_(dst, src, tab, rd, wr):
                sv = src[:].rearrange("p (h a b f) -> p h a b f", h=H, a=2, b=2)
                dv_ = dst[:].rearrange("p (h a b f) -> p h a b f", h=H, a=2, b=2)
                t1v = f1[:].rearrange("p (h a b f) -> p h a b f", h=H, a=2, b=2)
                tb = tab[:].rearrange("p (a b) f -> p a b f", a=2)
                for a in range(2):
                    cosb = tb[:, a, 0, :].unsqueeze(1).to_broadcast([128, H, 32])
                    sinb = tb[:, a, 1, :].unsqueeze(1).to_broadcast([128, H, 32])
                    p1, p2 = sv[:, :, a, 0, :], sv[:, :, a, 1, :]
                    S.op(S.dve, lambda e: e.tensor_tensor(out=t1v[:, :, a, 0, :], in0=p1, in1=cosb, op=ALU.mult), rd, [b_f1])
                    yield
                    S.op(S.pool, lambda e: e.tensor_tensor(out=t1v[:, :, a, 1, :], in0=p2, in1=sinb, op=ALU.mult), rd, [b_f1])
                    yield
                    S.op(S.dve, lambda e: e.tensor_tensor(out=dv_[:, :, a, 0, :], in0=t1v[:, :, a, 0, :], in1=t1v[:, :, a, 1, :], op=ALU.subtract), [b_f1], wr)
                    yield
                    S.op(S.pool, lambda e: e.tensor_tensor(out=t1v[:, :, a, 0, :], in0=p1, in1=sinb, op=ALU.mult), rd + wr, [b_f1])
                    yield
                    S.op(S.dve, lambda e: e.tensor_tensor(out=t1v[:, :, a, 1, :], in0=p2, in1=cosb, op=ALU.mult), rd, [b_f1])
                    yield
                    S.op(S.pool, lambda e: e.tensor_tensor(out=dv_[:, :, a, 1, :], in0=t1v[:, :, a, 0, :], in1=t1v[:, :, a, 1, :], op=ALU.add), [b_f1], wr)
                    yield

            def prep_ml(slot, ti, d):
                q, bq = inq[slot]; k, bk = ink[slot]; v, bv = inv[slot]; g, bg = ing[slot]
                S.op(S.dve, lambda e: e.tensor_tensor(out=sm[:, 0:16], in0=g[:], in1=gb[:], op=ALU.add), [bg, b_gb], [b_sm])
                yield
                gi = sm[:, d * 8:d * 8 + 4]
                gf = sm[:, d * 8 + 4:d * 8 + 8]
                yield from exp_(sm[:, 16:20], gf, [b_sm], [b_sm], scale=-1.0)
                S.op(S.act, lambda e: e.activation(out=sm[:, 20:24], in_=sm[:, 16:20], func=AF.Ln, bias=1.0), [b_sm], [b_sm])
                yield
                S.op(S.dve, lambda e: e.tensor_scalar(out=lf[:, 0:4], in0=sm[:, 20:24], scalar1=-1.0, scalar2=None, op0=ALU.mult), [b_sm], [b_lf])
                yield
                S.op(S.pe, lambda e: e.matmul(pX[:, 0:4], lhsT=mx[:, d, :], rhs=lf[:, 0:4], start=True, stop=True), [b_mx, b_lf], [bX])
                yield
                for j in range(6):
                    S.op(S.pe, lambda e: e.matmul(pF[:, 256 + j * 4:256 + j * 4 + 4], lhsT=selr[:, d, j, :], rhs=lf[:, 0:4], start=True, stop=True),
                         [b_selr, b_lf], [bF], inc=(j == 5))
                    yield
                fc, bfc = fac[slot]
                yield from exp_(fc[:].rearrange("p h j -> p j h"), pF[:, 256:280].rearrange("p (j h) -> p j h", j=6), [bF], [bfc])
                yield from exp_(sm[:, 24:28], pX[:, 0:4], [bX], [b_sm])
                S.op(S.dve, lambda e: e.tensor_tensor(out=sm[:, 28:32], in0=gi, in1=pX[:, 0:4], op=ALU.subtract), [b_sm, bX], [b_sm])
                yield
                yield from exp_(sm[:, 32:36], sm[:, 28:32], [b_sm], [b_sm], bias=None)
                S.op(S.dve, lambda e: e.tensor_scalar(out=sm[:, 32:36], in0=sm[:, 32:36], scalar1=float(128 ** -0.5), scalar2=None, op0=ALU.mult), [b_sm], [b_sm])
                yield
                if ti >= 2:
                    yield from rope_(f2, q, inr[slot][0], [bq, inr[slot][1]], [b_f2])
                    yield from rope_(f3, k, inr[slot][0], [bk, inr[slot][1]], [b_f3])
                    qs, bqs, ks, bks = f2, b_f2, f3, b_f3
                else:
                    qs, bqs, ks, bks = q, bq, k, bk
                eq = sm[:, 24:28].unsqueeze(2).to_broadcast([128, H, 128])
                ek = sm[:, 32:36].unsqueeze(2).to_broadcast([128, H, 128])
                S.op(S.dve, lambda e: e.tensor_tensor(out=qt[slot][0][:].rearrange("p (h d) -> p h d", h=H),
                                                      in0=qs[:].rearrange("p (h d) -> p h d", h=H), in1=eq, op=ALU.mult), [bqs, b_sm], [qt[slot][1]])
                yield
                S.op(S.dve, lambda e: e.tensor_tensor(out=kk[slot][0][:].rearrange("p (h d) -> p h d", h=H),
                                                      in0=ks[:].rearrange("p (h d) -> p h d", h=H), in1=ek, op=ALU.mult), [bks, b_sm], [kk[slot][1]])
                yield
                S.op(S.pool, lambda e: e.tensor_copy(out=vv[slot][0][:, :, 0:128], in_=v[:].rearrange("p (h d) -> p h d", h=H)), [bv], [vv[slot][1]])
                yield

            def core(slot, ti, d, osb, b_osb):
                q_, bq_ = qt[slot]; k_, bk_ = kk[slot]; v_, bv_ = vv[slot]; T_, bT_ = qkT[slot]; fc, bfc = fac[slot]
                for h in range(H):
                    S.op(S.pe, lambda e: e.transpose(pT[:, h * 128:(h + 1) * 128], q_[:, h * 128:(h + 1) * 128], idb[:]), [bq_, b_id], [bT], inc=False)
                    yield
                for h in range(H):
                    S.op(S.pe, lambda e: e.transpose(pT[:, (4 + h) * 128:(5 + h) * 128], k_[:, h * 128:(h + 1) * 128], idb[:]), [bk_, b_id], [bT], inc=(h == H - 1))
                    yield
                S.op(S.act, lambda e: e.copy(out=T_[:].rearrange("p j t -> p (j t)"), in_=pT[:, :]), [bT], [bT_])
                yield
                for h in range(H):
                    S.op(S.pe, lambda e: e.matmul(pA[:, h * 128:(h + 1) * 128], lhsT=T_[:, 4 + h, :], rhs=T_[:, h, :], start=True, stop=True),
                         [bT_], [bA], inc=(h == H - 1))
                    yield
                S.op(S.pool, lambda e: e.memset(Am[:], 0.0), [], [b_Am])
                yield
                S.op(S.dve, lambda e: e.copy_predicated(out=Am[:], mask=am[:, d, :].bitcast(U32), data=pA[:, :]), [bA, b_am], [b_Am])
                yield
                for cc in ((0, 1) if d == 0 else (1, 0)):
                    rs = slice(cc * 64, (cc + 1) * 64)
                    S.op(S.dve, lambda e: e.tensor_tensor(out=Sp[:], in0=St[:], in1=fc[:, :, cc * 3:cc * 3 + 1].to_broadcast([128, H, DV]), op=ALU.mult),
                         [b_St, bfc], [b_Sp])
                    yield
                    for h in range(H):
                        S.op(S.pe, lambda e: e.matmul(pOv[h // 2][rs, h % 2, :], lhsT=Am[rs, h * 128 + cc * 64:h * 128 + (cc + 1) * 64], rhs=v_[rs, h, :], start=True, stop=False),
                             [b_Am, bv_], [bO[h // 2]], inc=False)
                        yield
                        S.op(S.pe, lambda e: e.matmul(pOv[h // 2][rs, h % 2, :], lhsT=T_[:, h, rs], rhs=Sp[:, h, :], start=False, stop=True),
                             [bT_, b_Sp], [bO[h // 2]], inc=(h % 2 == 1))
                        yield
                    for h in range(H):
                        S.op(S.pe, lambda e: e.matmul(pKv[h // 2][:, h % 2, :], lhsT=k_[rs, h * 128:(h + 1) * 128], rhs=v_[rs, h, :], start=True, stop=True),
                             [bk_, bv_], [bK[h // 2]], inc=(h % 2 == 1))
                        yield
                    for j in range(2):
                        S.op(S.dve, lambda e: e.tensor_tensor(out=kvt[:, 2 * j:2 * j + 2, :], in0=pKv[j][:, :, :],
                                                              in1=fc[:, 2 * j:2 * j + 2, cc * 3 + 2:cc * 3 + 3].to_broadcast([128, 2, DV]), op=ALU.mult),
                             [bK[j], bfc], [b_kvt])
                        yield
                    S.op(S.pool, lambda e: e.tensor_tensor(out=St[:], in0=St[:], in1=fc[:, :, cc * 3 + 1:cc * 3 + 2].to_broadcast([128, H, DV]), op=ALU.mult),
                         [b_St, bfc], [b_St])
                    yield
                    S.op(S.dve, lambda e: e.tensor_tensor(out=St[:], in0=St[:], in1=kvt[:], op=ALU.add), [b_St, b_kvt], [b_St])
                    yield
                if kind == "hg":
                    for j in range(2):
                        S.op(S.act, lambda e: e.copy(out=osb[:, j * 256:(j + 1) * 256], in_=pO[j][:, 0:256]), [bO[j]], [b_osb])
                        yield
                else:
                    for j in range(2):
                        den = pOv[j][:, :, 128:129]
                        S.op(S.dve, lambda e: e.tensor_scalar(out=sc_[:, 36 + 2 * j:38 + 2 * j].unsqueeze(2), in0=den, scalar1=-1.0, scalar2=None, op0=ALU.mult),
                             [bO[j]], [b_sc])
                        yield
                        S.op(S.dve, lambda e: e.scalar_tensor_tensor(out=sc_[:, 40 + 2 * j:42 + 2 * j].unsqueeze(2), in0=den, scalar=1.0,
                                                                     in1=sc_[:, 36 + 2 * j:38 + 2 * j].unsqueeze(2), op0=ALU.max, op1=ALU.max),
                             [bO[j], b_sc], [b_sc])
                        yield
                    S.op(S.dve, lambda e: e.reciprocal(out=sc_[:, 44:48], in_=sc_[:, 40:44]), [b_sc], [b_sc])
                    yield
                    for j in range(2):
                        S.op(S.dve, lambda e: e.tensor_tensor(out=osb[:, j * 256:(j + 1) * 256].rearrange("p (h d) -> p h d", h=2), in0=pOv[j][:, :, 0:128],
                                                              in1=sc_[:, 44 + 2 * j:46 + 2 * j].unsqueeze(2).to_broadcast([128, 2, 128]), op=ALU.mult), [bO[j], b_sc], [b_osb])
                        yield

            def readout(slot, ti, osb, b_osb):
                g2, bg2 = ing2[slot]; of_, bof = inof[slot]
                S.op(S.dve, lambda e: e.tensor_tensor(out=ro1[:], in0=osb[:], in1=of_[:], op=ALU.add), [b_osb, bof], [b_ro1])
                yield
                for h in range(H):
                    S.op(S.act, lambda e: e.activation(out=ro2[:, h * 128:(h + 1) * 128], in_=ro1[:, h * 128:(h + 1) * 128], func=AF.Square,
                                                       accum_out=sr[:, 48 + h:49 + h]), [b_ro1], [b_ro2, b_sr])
                    yield
                S.op(S.dve, lambda e: e.tensor_scalar(out=sr[:, 52:56], in0=sr[:, 48:52], scalar1=1.0 / 128, scalar2=EPS, op0=ALU.mult, op1=ALU.add), [b_sr], [b_sr])
                yield
                S.op(S.act, lambda e: e.activation(out=sr[:, 56:60], in_=sr[:, 52:56], func=AF.Ln), [b_sr], [b_sr])
                yield
                yield from exp_(sr[:, 60:64], sr[:, 56:60], [b_sr], [b_sr], scale=-0.5)
                S.op(S.dve, lambda e: e.tensor_tensor(out=ro2[:].rearrange("p (h d) -> p h d", h=H), in0=ro1[:].rearrange("p (h d) -> p h d", h=H),
                                                      in1=sr[:, 60:64].unsqueeze(2).to_broadcast([128, H, 128]), op=ALU.mult), [b_ro1, b_sr], [b_ro2])
                yield
                S.op(S.pool, lambda e: e.tensor_tensor(out=ro2[:], in0=ro2[:], in1=nw[:], op=ALU.mult), [b_ro2, b_nw], [b_ro2])
                yield
                yield from exp_(ro1[:], g2[:], [bg2], [b_ro1], scale=-1.0)
                S.op(S.act, lambda e: e.activation(out=ro1[:], in_=ro1[:], func=AF.Ln, bias=1.0), [b_ro1], [b_ro1])
                yield
                yield from exp_(ro1[:], ro1[:], [b_ro1], [b_ro1], scale=-1.0)
                if kind == "hg":
                    S.op(S.pool, lambda e: e.tensor_tensor(out=ro1[:], in0=ro1[:], in1=g2[:], op=ALU.mult), [b_ro1, bg2], [b_ro1])
                    yield
                S.op(S.dve, lambda e: e.tensor_tensor(out=rob[:], in0=ro2[:], in1=ro1[:], op=ALU.mult), [b_ro1, b_ro2], [b_rob])
                yield
                pRb = pR[:].bitcast(BF16)
                for j in range(4):
                    S.op(S.pe, lambda e: e.transpose(pRb[:, j * 128:(j + 1) * 128], rob[:, j * 128:(j + 1) * 128], idb[:]), [b_rob, b_id], [bR], inc=(j == 3))
                    yield
                S.op(S.act, lambda e: e.copy(out=brs[:].rearrange("p j t -> p (j t)"), in_=pRb[:, 0:512]), [bR], [b_brs])
                yield
                S.dma(S.sp, lambda e: e.dma_start(out=dr["brT"][br_idx, :, ti * 128:(ti + 1) * 128].rearrange("(j p) t -> p j t", p=128), in_=brs[:]),
                      [b_brs], [B["brT"]])
                yield

            prep = prep_hg if kind == "hg" else prep_ml

            def chain(*gens):
                for g in gens:
                    yield from g

            def store_fwd(ti, osb, b_osb):
                S.dma(S.sp, lambda e: e.dma_start(out=dr[of_name][ti * 128:(ti + 1) * 128, :], in_=osb[:]), [b_osb], [B[of_name]])
                yield

            def interleave(*gens):
                gens = [iter(g) for g in gens]
                done = [False] * len(gens)
                while not all(done):
                    for k, g in enumerate(gens):
                        if not done[k]:
                            try:
                                next(g)
                            except StopIteration:
                                done[k] = True

            for d in range(2):
                order = list(range(NT)) if d == 0 else [1, 0] + list(range(NT - 1, 1, -1))
                S.op(S.dve, lambda e: e.memset(St[:], 0.0), [], [b_St])
                load(0, order[0], d)
                if len(order) > 1:
                    load(1, order[1], d)
                for _ in prep(0, order[0], d):
                    pass
                pend = iter(())
                for i, ti in enumerate(order):
                    slot = i % NB
                    osb, b_osb = osbs[i % 2]
                    work = [core(slot, ti, d, osb, b_osb)]
                    if d == 0:
                        work.append(store_fwd(ti, osb, b_osb))
                    nxt = prep((i + 1) % NB, order[i + 1], d) if i + 1 < len(order) else iter(())
                    interleave(nxt, chain(*work), pend)
                    if i + 2 < len(order):
                        load((i + 2) % NB, order[i + 2], d)
                    if d == 1 and not (last and ti < 2):
                        pend = readout(slot, ti, osb, b_osb)
                    else:
                        pend = iter(())
                for _ in pend:
                    pass
            S.barrier()

    def declare_na(self):
        self.din("nabias", [DEPTH, 5, 8, 128, 5, 128])

    def stage_na(self, layer, last):
        nc, S, B, ps, psb, dr = self.nc, self.S, self.bufs, self.ps, self.psb, self.dram
        with ExitStack() as c:
            idb, b_id = self.TB(c, "a_id", [128, 128], BF16)
            S.dma(S.pool, lambda e: e.dma_start(out=idb[:], in_=dr["ident"][:, :]), [B["ident"]], [b_id])
            kT, _ = self.TB(c, "a_kT", [128, 4, T], BF16)
            KCH = 8
            b_kT = [Buf(f"kT{i}") for i in range((NT + KCH - 1) // KCH)]
            vN, _ = self.TB(c, "a_vN", [128, NT, 8, 65], BF16)
            b_vN = [Buf(f"vN{i}") for i in range(NT)]
            S.op(S.dve, lambda e: e.memset(vN[:, :, :, 64:65], 1.0), [], b_vN)

            def load_k(ci):
                a_, b_ = ci * KCH * 128, min(NT, (ci + 1) * KCH) * 128
                S.dma(S.sp, lambda e: e.dma_start(out=kT[:, :, a_:b_], in_=dr["p_naqk"][512:1024, a_:b_].rearrange("(j p) t -> p j t", p=128)),
                      [B["p_naqk"]], [b_kT[ci]])

            def load_v(t_):
                S.dma(S.sp, lambda e: e.dma_start(out=vN[:, t_, :, 0:64],
                                                  in_=dr["p_nav"][t_ * 128:(t_ + 1) * 128, :].rearrange("p (h d) -> p h d", h=8)),
                      [B["p_nav"]], [b_vN[t_]])

            for ci in range(len(b_kT)):
                load_k(ci)
                for t_ in range(ci * KCH, min(NT, (ci + 1) * KCH)):
                    load_v(t_)
            bias_i, b_bi = self.TB(c, "a_bi", [128, 8, 640], F32)
            S.dma(S.sp, lambda e: e.dma_start(out=bias_i[:].rearrange("p h (j q) -> p h j q", j=5),
                                                in_=dr["nabias"][layer, 0].rearrange("h p j q -> p h j q")), [B["nabias"]], [b_bi])
            bias_e, b_be = self.TB(c, "a_be", [128, 8, 640], F32)
            qq = [self.TB(c, f"a_qq{i}", [128, 4, 128], BF16) for i in range(2)]
            sc = [self.TB(c, f"a_sc{i}", [128, 640], F32) for i in range(3)]
            pt = [self.TB(c, f"a_pt{i}", [128, 896], BF16) for i in range(3)]
            rc, b_rc = self.TB(c, "a_rc", [128, 8], F32)
            no, b_no = self.TB(c, "a_no", [128, 512], BF16)
            brs, b_brs = self.TB(c, "a_brs", [128, 4, 128], BF16)
            pS = [self.psall[:, 0:1024], self.psall[:, 1024:2048], self.psall[:, 2048:3072]]
            bS = [Buf("naS0"), Buf("naS1"), Buf("naS2")]
            pO = [ps[7][:, 0:128], ps[7][:, 128:256], ps[7][:, 256:384]]
            bO = [Buf("naO0"), Buf("naO1"), Buf("naO2")]
            pR, bR = ps[6][:].bitcast(BF16), psb[6]
            qbs = ([] if last else [("ctx", 0), ("ctx", 1)]) + [("lat", i) for i in range(32)]
            it = 0

            def loadq(i):
                kind, qb = qbs[i]
                ti = qb if kind == "ctx" else 2 + qb
                S.dma(S.sp, lambda e: e.dma_start(out=qq[i % 2][0][:], in_=dr["p_naqk"][0:512, ti * 128:(ti + 1) * 128].rearrange("(j p) t -> p j t", p=128)),
                      [B["p_naqk"]], [qq[i % 2][1]])

            loadq(0)
            for i, (kind, qb) in enumerate(qbs):
                if i + 1 < len(qbs):
                    loadq(i + 1)
                q_, bq_ = qq[i % 2]
                ti = qb if kind == "ctx" else 2 + qb
                if kind == "lat":
                    R0 = min(max(2 * qb - 4, 0), 54)
                    kt0 = 2 + R0 // 2
                    cls = {0: 1, 1: 2, 30: 3, 31: 4}.get(qb, 0)
                    if cls:
                        S.dma(S.sp, lambda e: e.dma_start(out=bias_e[:].rearrange("p h (j q) -> p h j q", j=5),
                                                            in_=dr["nabias"][layer, cls].rearrange("h p j q -> p h j q")), [B["nabias"]], [b_be])
                        bt, bbt = bias_e, b_be
                    else:
                        bt, bbt = bias_i, b_bi
                    kts = [kt0 + j for j in range(5)] + [0, 1]
                else:
                    kts = [0, 1]
                nk = len(kts)
                for hp in range(4):
                    slots = []
                    for hh in range(2):
                        slots.append((pS[it % 3], bS[it % 3], sc[it % 3], pt[it % 3], pO[it % 3], bO[it % 3]))
                        it += 1
                    for j, kt in enumerate(kts):
                        for hh in range(2):
                            base = hh * 64
                            s_, bs_ = slots[hh][0], slots[hh][1]
                            S.op(S.pe, lambda e: e.matmul(s_[:, j * 128:(j + 1) * 128], lhsT=kT[base:base + 64, hp, kt * 128:(kt + 1) * 128],
                                                          rhs=q_[base:base + 64, hp, :], start=True, stop=True), [b_kT[kt // KCH], bq_], [bs_], inc=(j == nk - 1))
                    for hh in range(2):
                        h = 2 * hp + hh
                        s_, bs_, (sc_, bsc_), (p_, bp_), o_, bo_ = slots[hh]
                        if kind == "lat":
                            S.op(S.dve, lambda e: e.scalar_tensor_tensor(out=sc_[:], in0=s_[:, 0:640], scalar=0.125, in1=bt[:, h, :], op0=ALU.mult, op1=ALU.add),
                                 [bs_, bbt], [bsc_])
                            S.op(S.act, lambda e: e.activation(out=p_[:, 0:640], in_=sc_[:], func=AF.Exp), [bsc_], [bp_])
                            S.op(S.act, lambda e: e.activation(out=p_[:, 640:896], in_=s_[:, 640:896], func=AF.Exp, scale=0.125), [bs_], [bp_])
                        else:
                            S.op(S.act, lambda e: e.activation(out=p_[:, 0:256], in_=s_[:, 0:256], func=AF.Exp, scale=0.125), [bs_], [bp_])
                        for j, kt in enumerate(kts):
                            S.op(S.pe, lambda e: e.matmul(o_[:, 0:65], lhsT=p_[:, j * 128:(j + 1) * 128], rhs=vN[:, kt, h, :], start=(j == 0), stop=(j == nk - 1)),
                                 [bp_, b_vN[kt]], [bo_], inc=(j == nk - 1))
                        S.op(S.dve, lambda e: e.reciprocal(out=rc[:, h:h + 1], in_=o_[:, 64:65]), [bo_], [b_rc])
                        S.op(S.act, lambda e: e.activation(out=no[:, h * 64:(h + 1) * 64], in_=o_[:, 0:64], func=AF.Copy, scale=rc[:, h:h + 1]), [bo_, b_rc], [b_no])
                for j in range(4):
                    S.op(S.pe, lambda e: e.transpose(pR[:, j * 128:(j + 1) * 128], no[:, j * 128:(j + 1) * 128], idb[:]), [b_no, b_id], [bR], inc=(j == 3))
                S.op(S.dve, lambda e: e.tensor_copy(out=brs[:].rearrange("p j t -> p (j t)"), in_=pR[:, 0:512]), [bR], [b_brs])
                S.dma(S.sp, lambda e: e.dma_start(out=dr["brT"][0, :, ti * 128:(ti + 1) * 128].rearrange("(j p) t -> p j t", p=128), in_=brs[:]),
                      [b_brs], [B["brT"]])
            S.barrier()


    def declare_tail(self):
        self.din("w_branch", [DEPTH, 3, 512, D])
        self.din("w_out", [DEPTH, D, D])
        self.din("ffn_w_up", [1, D, 2 * FFN])
        self.din("ffn_w_down", [1, FFN, D])
        self.din("moe_router_t", [1, NEXP, D])
        self.din("moe_w_up", [1, NEXP, D, 2 * EDIM])
        self.din("moe_w_down", [1, NEXP, EDIM, D])
        self.din("final_norm_w", [D])
        self.dscr("glu_acc", [T, D])
        self.acc_bufs = [Buf(f"acc{i}") for i in range(NT)]
        self.h_bufs = [Buf(f"hb{i}") for i in range(NT)]
        self.dout("out", [SEQ // 2, D])
        self.din("tokidx", [128, 16], U32)

    def stage_merge(self, layer, aT, aT_bufs, last):
        nc, S, B, ps, psb, dr = self.nc, self.S, self.bufs, self.ps, self.psb, self.dram
        t0 = 2 if last else 0
        hsrc = "xin" if layer == 0 else "hbuf"
        with ExitStack() as c:
            wg, _ = self.TB(c, "g_wg", [128, KC, 3072], BF16)
            b_wg = [Buf(f"wg{j}") for j in range(6)]
            wb, _ = self.TB(c, "g_wb", [128, 12, D], BF16)
            b_wb = [Buf(f"wb{i}") for i in range(3)]
            wo, _ = self.TB(c, "g_wo", [128, KC, D], BF16)
            b_wo = [Buf("wo0"), Buf("wo1")]

            def ld_wg(j):
                S.dma(S.pool, lambda e: e.dma_start(out=wg[:, :, j * 512:(j + 1) * 512],
                                                    in_=dr["w_in"][layer, :, C_BG + j * 512:C_BG + (j + 1) * 512].rearrange("(kc p) n -> p kc n", p=128)),
                      [B["w_in"]], [b_wg[j]])

            def ld_wb(i):
                S.dma(S.pool, lambda e: e.dma_start(out=wb[:, i * 4:(i + 1) * 4, :], in_=dr["w_branch"][layer, i].rearrange("(kc p) n -> p kc n", p=128)),
                      [B["w_branch"]], [b_wb[i]])

            def ld_wo(j):
                S.dma(S.pool, lambda e: e.dma_start(out=wo[:, :, j * 512:(j + 1) * 512],
                                                    in_=dr["w_out"][layer, :, j * 512:(j + 1) * 512].rearrange("(kc p) n -> p kc n", p=128)),
                      [B["w_out"]], [b_wo[j]])

            for i in range(3):
                ld_wg(2 * i)
                ld_wb(i)
            for i in range(3):
                ld_wg(2 * i + 1)
            ld_wo(0)
            ld_wo(1)
            g1 = [self.TB(c, f"g_g1{r}", [128, D], F32) for r in range(2)]
            for r in range(2):
                self.load_row_bcast(g1[r][0][:], layer, r, 2, g1[r][1])
            br = [self.TB(c, "g_br0", [128, 12, 512], BF16)] * 2
            sg = [self.TB(c, f"g_sg{i}", [128, 512], F32) for i in range(2)]
            tm = [self.TB(c, f"g_tm{i}", [128, 512], F32) for i in range(2)]
            ya, b_ya = self.TB(c, "g_ya", [128, 512], F32)
            yT, b_yT = self.TB(c, "g_yT", [128, KC, 512], BF16)
            hin = [self.TB(c, "g_hin0", [128, D], F32)] * 2
            ho = [self.TB(c, "g_ho0", [128, D], F32)] * 2
            blocks = [(tb, min(4, NT - tb)) for tb in range(t0, NT, 4)]

            def loadbr(bi):
                tb, nt_ = blocks[bi]
                for i in range(3):
                    S.dma(S.sp, lambda e: e.dma_start(out=br[bi % 2][0][:, i * 4:(i + 1) * 4, 0:nt_ * 128],
                                                      in_=dr["brT"][i, :, tb * 128:(tb + nt_) * 128].rearrange("(kc p) t -> p kc t", p=128)),
                          [B["brT"]], [br[bi % 2][1]])

            cnt = 0
            hc = 0
            loadbr(0)
            for bi, (tb, nt_) in enumerate(blocks):
                n = nt_ * 128
                tok0 = tb * 128
                br_, bbr_ = br[bi % 2]
                for m in range(8):
                    for i in range(3):
                        pG, bG = ps[cnt % 2], psb[cnt % 2]
                        pZ, bZ = ps[2 + cnt % 2], psb[2 + cnt % 2]
                        sg_, bsg_ = sg[cnt % 2]
                        tm_, btm_ = tm[cnt % 2]
                        cnt += 1
                        for k in range(KC):
                            S.op(S.pe, lambda e: e.matmul(pG[:, 0:n], lhsT=wg[:, k, i * 1024 + m * 128:i * 1024 + (m + 1) * 128], rhs=aT[:, k, tok0:tok0 + n],
                                                          start=(k == 0), stop=(k == KC - 1)), [b_wg[(i * 1024 + m * 128) // 512]] + [aT_bufs[t] for t in range(tb, tb + nt_)], [bG], inc=(k == KC - 1))
                        for k in range(4):
                            S.op(S.pe, lambda e: e.matmul(pZ[:, 0:n], lhsT=wb[:, i * 4 + k, m * 128:(m + 1) * 128], rhs=br_[:, i * 4 + k, 0:n],
                                                          start=(k == 0), stop=(k == 3)), [b_wb[i], bbr_], [bZ], inc=(k == 3))
                        S.op(S.act, lambda e: e.activation(out=sg_[:, 0:n], in_=pG[:, 0:n], func=AF.Sigmoid), [bG], [bsg_])
                        if i == 0:
                            S.op(S.dve, lambda e: e.tensor_tensor(out=ya[:, 0:n], in0=pZ[:, 0:n], in1=sg_[:, 0:n], op=ALU.mult), [bZ, bsg_], [b_ya])
                        else:
                            S.op(S.dve, lambda e: e.tensor_tensor(out=tm_[:, 0:n], in0=pZ[:, 0:n], in1=sg_[:, 0:n], op=ALU.mult), [bZ, bsg_], [btm_])
                            if i == 1:
                                S.op(S.pool, lambda e: e.tensor_tensor(out=ya[:, 0:n], in0=ya[:, 0:n], in1=tm_[:, 0:n], op=ALU.add), [b_ya, btm_], [b_ya])
                            else:
                                S.op(S.pool, lambda e: e.tensor_tensor(out=yT[:, m, 0:n], in0=ya[:, 0:n], in1=tm_[:, 0:n], op=ALU.add), [b_ya, btm_], [b_yT])
                if bi + 1 < len(blocks):
                    loadbr(bi + 1)
                for j in range(nt_):
                    ti = tb + j
                    r = 1 if ti < 2 else 0
                    hi_, bhi_ = hin[hc % 2]
                    ho_, bho_ = ho[hc % 2]
                    hc += 1
                    S.dma(S.sp, lambda e: e.dma_start(out=hi_[:], in_=dr[hsrc][ti * 128:(ti + 1) * 128, :]), [B[hsrc], self.h_bufs[ti]], [bhi_])
                    for half in range(2):
                        pW, bW = ps[4 + (2 * hc + half) % 4], psb[4 + (2 * hc + half) % 4]
                        for k in range(KC):
                            S.op(S.pe, lambda e: e.matmul(pW[:, :], lhsT=yT[:, k, j * 128:(j + 1) * 128], rhs=wo[:, k, half * 512:(half + 1) * 512],
                                                          start=(k == 0), stop=(k == KC - 1)), [b_yT, b_wo[half]], [bW], inc=(k == KC - 1))
                        S.op(S.dve, lambda e: e.tensor_tensor(out=ho_[:, half * 512:(half + 1) * 512], in0=pW[:, :], in1=g1[r][0][:, half * 512:(half + 1) * 512],
                                                              op=ALU.mult), [bW, g1[r][1]], [bho_])
                    S.op(S.pool, lambda e: e.tensor_tensor(out=ho_[:], in0=ho_[:], in1=hi_[:], op=ALU.add), [bho_, bhi_], [bho_])
                    S.dma(S.sp, lambda e: e.dma_start(out=dr["hbuf"][ti * 128:(ti + 1) * 128, :], in_=ho_[:]), [bho_], [self.h_bufs[ti]])
            S.barrier()

    def stage_glu(self, layer, fT, fT_bufs, passes, t0, gates=None, ntile=None, resid=False):
        nc, S, B, ps, psb, dr = self.nc, self.S, self.bufs, self.ps, self.psb, self.dram
        ntile = NT - t0 if ntile is None else ntile
        with ExitStack() as c:
            NCH = max(p["nch"] for p in passes)
            wa = [self.TB(c, f"u_wa{i}", [128, KC, NCH * 128], BF16) for i in range(2)]
            wu = [self.TB(c, f"u_wu{i}", [128, KC, NCH * 128], BF16) for i in range(2)]
            wd = [self.TB(c, f"u_wd{i}", [128, NCH, D], BF16) for i in range(2)]
            sA = [self.TB(c, f"u_sA{i}", [128, 512], BF16) for i in range(2)]
            hm = [self.TB(c, f"u_hm{i}", [128, NCH, 512], BF16) for i in range(2)]
            stg = [self.TB(c, f"u_st{i}", [128, D], F32) for i in range(2)]
            acc = [self.TB(c, f"u_ac{i}", [128, D], F32) for i in range(2)]
            wst = [self.TB(c, f"u_ws{i}", [128, 2688], F32) for i in range(2)]
            if resid:
                g5 = [self.TB(c, f"u_g5{r}", [128, D], F32) for r in range(2)]
                for r in range(2):
                    self.load_row_bcast(g5[r][0][:], layer, r, 5, g5[r][1])
                hres = [self.TB(c, f"u_hr{i}", [128, D], F32) for i in range(2)]
            blocks = [(tb, min(4, t0 + ntile - tb)) for tb in range(t0, t0 + ntile, 4)]
            nblk = len(blocks)
            wcnt = [0]

            def pieces(pi):
                p = passes[pi]
                nch, c0, hid, up, down = p["nch"], p["c0"], p["hid"], p["up"], p["down"]
                i = pi % 2
                out = []
                for (w_, bw_), off in ((wa[i], 0), (wu[i], hid)):
                    for k0 in range(0, KC, 3):
                        kn = min(3, KC - k0)
                        out.append((w_[:, k0:k0 + kn, 0:nch * 128], bw_, up[k0 * 128:(k0 + kn) * 128, off + c0 * 128:off + (c0 + nch) * 128].rearrange("(kc p) n -> p kc n", p=128),
                                    p["upn"], (kn, nch * 128)))
                for j0 in range(0, nch, 2):
                    jn = min(2, nch - j0)
                    out.append((wd[i][0][:, j0:j0 + jn, :], wd[i][1], down[(c0 + j0) * 128:(c0 + j0 + jn) * 128, :].rearrange("(j p) n -> p j n", p=128),
                                p["downn"], (jn, D)))
                return out

            def load_piece(pc):
                dst, bdst, src, srcn, (a, b) = pc
                st_, bst_ = wst[wcnt[0] % 2]
                wcnt[0] += 1
                view = st_[:, 0:a * b].rearrange("p (a b) -> p a b", a=a)
                S.dma(S.sp, lambda e: e.dma_start(out=view, in_=src), [B[srcn]], [bst_])
                if wcnt[0] % 2 == 0:
                    S.op(S.act, lambda e: e.copy(out=dst, in_=view), [bst_], [bdst])
                else:
                    S.op(S.pool, lambda e: e.tensor_copy(out=dst, in_=view), [bst_], [bdst])

            for pc in pieces(0):
                load_piece(pc)
            cnt = 0
            for pi, p in enumerate(passes):
                nxt = pieces(pi + 1) if pi + 1 < len(passes) else []
                nch = p["nch"]
                (wa_, bwa_), (wu_, bwu_), (wd_, bwd_) = wa[pi % 2], wu[pi % 2], wd[pi % 2]
                for bi, (tb, nt_) in enumerate(blocks):
                    lo, hi = (bi * len(nxt)) // nblk, ((bi + 1) * len(nxt)) // nblk
                    for pc in nxt[lo:hi]:
                        load_piece(pc)
                    n = nt_ * 128
                    tok0 = tb * 128
                    hm_, bhm_ = hm[bi % 2]
                    fb = [fT_bufs[t] for t in range(tb, tb + nt_)]
                    for jc in range(nch):
                        pA, bA = ps[cnt % 2], psb[cnt % 2]
                        pU, bU = ps[2 + cnt % 2], psb[2 + cnt % 2]
                        sA_, bsA_ = sA[cnt % 2]
                        cnt += 1
                        for k in range(KC):
                            S.op(S.pe, lambda e: e.matmul(pA[:, 0:n], lhsT=wa_[:, k, jc * 128:(jc + 1) * 128], rhs=fT[:, k, tok0:tok0 + n],
                                                          start=(k == 0), stop=(k == KC - 1)), [bwa_] + fb, [bA], inc=(k == KC - 1))
                        for k in range(KC):
                            S.op(S.pe, lambda e: e.matmul(pU[:, 0:n], lhsT=wu_[:, k, jc * 128:(jc + 1) * 128], rhs=fT[:, k, tok0:tok0 + n],
                                                          start=(k == 0), stop=(k == KC - 1)), [bwu_] + fb, [bU], inc=(k == KC - 1))
                        S.op(S.act, lambda e: e.activation(out=sA_[:, 0:n], in_=pA[:, 0:n], func=AF.Silu), [bA], [bsA_])
                        S.op(S.dve, lambda e: e.tensor_tensor(out=hm_[:, jc, 0:n], in0=pU[:, 0:n], in1=sA_[:, 0:n], op=ALU.mult), [bU, bsA_], [bhm_])
                    for j in range(nt_):
                        ti = tb + j
                        st_, bst_ = stg[cnt % 2]
                        ac_, bac_ = acc[cnt % 2]
                        fin = resid and pi == len(passes) - 1
                        if fin:
                            hr_, bhr_ = hres[cnt % 2]
                            S.dma(S.sp, lambda e: e.dma_start(out=hr_[:], in_=dr["hbuf"][ti * 128:(ti + 1) * 128, :]), [self.h_bufs[ti]], [bhr_])
                        if pi > 0:
                            S.dma(S.sp, lambda e: e.dma_start(out=ac_[:], in_=dr["glu_acc"][ti * 128:(ti + 1) * 128, :]), [self.acc_bufs[ti]], [bac_])
                        for half in range(2):
                            hs = slice(half * 512, (half + 1) * 512)
                            pD, bD = ps[4 + (2 * cnt + half) % 4], psb[4 + (2 * cnt + half) % 4]
                            for jc in range(nch):
                                S.op(S.pe, lambda e: e.matmul(pD[:, :], lhsT=hm_[:, jc, j * 128:(j + 1) * 128], rhs=wd_[:, jc, hs],
                                                              start=(jc == 0), stop=(jc == nch - 1)), [bhm_, bwd_], [bD], inc=(jc == nch - 1))
                            if gates is not None:
                                gs = gates[0][:, ti, p["gate"]:p["gate"] + 1]
                                if pi == 0:
                                    S.op(S.act, lambda e: e.activation(out=st_[:, hs], in_=pD[:, :], func=AF.Copy, scale=gs), [bD, gates[1]], [bst_])
                                else:
                                    S.op(S.dve, lambda e: e.scalar_tensor_tensor(out=st_[:, hs], in0=pD[:, :], scalar=gs, in1=ac_[:, hs], op0=ALU.mult, op1=ALU.add),
                                         [bD, gates[1], bac_], [bst_])
                            elif pi == 0:
                                S.op(S.act, lambda e: e.copy(out=st_[:, hs], in_=pD[:, :]), [bD], [bst_])
                            else:
                                S.op(S.dve, lambda e: e.tensor_tensor(out=st_[:, hs], in0=pD[:, :], in1=ac_[:, hs], op=ALU.add), [bD, bac_], [bst_])
                        cnt += 1
                        if fin:
                            r_ = 1 if ti < 2 else 0
                            S.op(S.dve, lambda e: e.tensor_tensor(out=st_[:], in0=st_[:], in1=g5[r_][0][:], op=ALU.mult), [bst_, g5[r_][1]], [bst_])
                            S.op(S.pool, lambda e: e.tensor_tensor(out=st_[:], in0=st_[:], in1=hr_[:], op=ALU.add), [bst_, bhr_], [bst_])
                            S.dma(S.sp, lambda e: e.dma_start(out=dr["hbuf"][ti * 128:(ti + 1) * 128, :], in_=st_[:]), [bst_], [self.h_bufs[ti]])
                        else:
                            S.dma(S.sp, lambda e: e.dma_start(out=dr["glu_acc"][ti * 128:(ti + 1) * 128, :], in_=st_[:]), [bst_], [self.acc_bufs[ti]])
            S.barrier()

    def stage_final(self, layer, last, gather=None):
        nc, S, B, dr = self.nc, self.S, self.bufs, self.dram
        t0 = 2 if last else 0
        with ExitStack() as c:
            g5 = [self.TB(c, f"f_g5{r}", [128, D], F32) for r in range(2)]
            for r in range(2):
                self.load_row_bcast(g5[r][0][:], layer, r, 5, g5[r][1])
            fw, b_fw = self.TB(c, "f_fw", [128, D], F32)
            S.dma(S.sp, lambda e: e.dma_start(out=fw[:], in_=dr["final_norm_w"].rearrange("(o n) -> o n", o=1).partition_broadcast(128)), [B["final_norm_w"]], [b_fw])
            hi = [self.TB(c, f"f_hi{i}", [128, D], F32) for i in range(2)]
            ac = [self.TB(c, f"f_ac{i}", [128, D], F32) for i in range(2)]
            ot = [self.TB(c, f"f_ot{i}", [128, D], F32) for i in range(2)]
            sq, b_sq = self.TB(c, "f_sq", [128, D], BF16)
            st = [self.TB(c, f"f_st{i}", [128, 4], F32) for i in range(2)]
            tiles = list(range(t0, NT)) if gather is None else list(range(2, 2 + 16))

            def load(i):
                ti = tiles[i]
                if gather is None:
                    S.dma(S.sp, lambda e: e.dma_start(out=hi[i % 2][0][:], in_=dr["hbuf"][ti * 128:(ti + 1) * 128, :]), [self.h_bufs[ti]], [hi[i % 2][1]])
                else:
                    S.dma(S.pool, lambda e: e.indirect_dma_start(out=hi[i % 2][0][:], out_offset=None, in_=dr["hbuf"][:, :],
                                                                 in_offset=bass.IndirectOffsetOnAxis(ap=gather[0][:, ti - 2:ti - 1], axis=0)),
                          [B["hbuf"], gather[1]] + self.h_bufs, [hi[i % 2][1]])
                S.dma(S.sp, lambda e: e.dma_start(out=ac[i % 2][0][:], in_=dr["glu_acc"][ti * 128:(ti + 1) * 128, :]), [self.acc_bufs[ti]], [ac[i % 2][1]])

            load(0)
            for i, ti in enumerate(tiles):
                if i + 1 < len(tiles):
                    load(i + 1)
                r = 1 if ti < 2 else 0
                (h_, bh_), (a_, ba_), (o_, bo_), (s_, bs_) = hi[i % 2], ac[i % 2], ot[i % 2], st[i % 2]
                S.op(S.dve, lambda e: e.tensor_tensor(out=a_[:], in0=a_[:], in1=g5[r][0][:], op=ALU.mult), [ba_, g5[r][1]], [ba_])
                S.op(S.pool, lambda e: e.tensor_tensor(out=o_[:], in0=a_[:], in1=h_[:], op=ALU.add), [ba_, bh_], [bo_])
                if not last:
                    S.dma(S.sp, lambda e: e.dma_start(out=dr["hbuf"][ti * 128:(ti + 1) * 128, :], in_=o_[:]), [bo_], [self.h_bufs[ti]])
                else:
                    S.op(S.act, lambda e: e.activation(out=sq[:], in_=o_[:], func=AF.Square, accum_out=s_[:, 0:1]), [bo_], [b_sq, bs_])
                    S.op(S.dve, lambda e: e.tensor_scalar(out=s_[:, 1:2], in0=s_[:, 0:1], scalar1=1.0 / D, scalar2=EPS, op0=ALU.mult, op1=ALU.add), [bs_], [bs_])
                    S.op(S.act, lambda e: e.activation(out=s_[:, 2:3], in_=s_[:, 1:2], func=AF.Ln), [bs_], [bs_])
                    S.op(S.act, lambda e: e.activation(out=s_[:, 3:4], in_=s_[:, 2:3], func=AF.Exp, scale=-0.5), [bs_], [bs_])
                    S.op(S.dve, lambda e: e.scalar_tensor_tensor(out=o_[:], in0=o_[:], scalar=s_[:, 3:4], in1=fw[:], op0=ALU.mult, op1=ALU.mult), [bo_, bs_, b_fw], [bo_])
                    S.dma(S.sp, lambda e: e.dma_start(out=dr["out"][(ti - 2) * 128:(ti - 1) * 128, :], in_=o_[:]), [bo_], [B["out"]])
            S.barrier()

    def glu_passes(self, layer):
        dr = self.dram
        if layer % 2 == 0:
            i = layer // 2
            return [dict(up=dr["ffn_w_up"][i], down=dr["ffn_w_down"][i], upn="ffn_w_up", downn="ffn_w_down", hid=FFN, c0=c0, nch=n, gate=None)
                    for c0, n in ((0, 6), (6, 6), (12, 5), (17, 5))]
        i = layer // 2
        return [dict(up=dr["moe_w_up"][i, ex], down=dr["moe_w_down"][i, ex], upn="moe_w_up", downn="moe_w_down", hid=EDIM, c0=c0, nch=7, gate=ex)
                for ex in range(NEXP) for c0 in (0, 7, 14, 21)]

    def run_layer(self, layer, aT, aT_bufs, stop_after=None):
        S = self.S
        last = layer == DEPTH - 1
        src = "xin" if layer == 0 else "hbuf"
        if not getattr(self, "mods_done", False):
            self.stage_mod(layer)
        self.stage_norm(layer, 0, src, aT, aT_bufs, list(range(NT)))
        self.stage_inproj(layer, aT, aT_bufs)
        self.stage_na(layer, last)
        self.stage_scan(layer, "hg", last)
        self.stage_scan(layer, "ml", last)
        self.stage_merge(layer, aT, aT_bufs, last)
        if stop_after == "merge":
            return
        t0 = 2 if last else 0
        if layer % 2 == 0:
            self.stage_norm(layer, 1, "hbuf", aT, aT_bufs, list(range(t0, NT)))
            self.stage_glu(layer, aT, aT_bufs, self.glu_passes(layer), t0, resid=(not last))
            if not last:
                return
        else:
            with ExitStack() as c:
                gates, b_gates = self.TB(c, "r_gates", [128, NT, NEXP], F32)
                tix, b_tix = self.TB(c, "r_tix", [128, 16], U32)
                S.dma(S.sp, lambda e: e.dma_start(out=tix[:], in_=self.dram["tokidx"][:, :]), [self.bufs["tokidx"]], [b_tix])
                half_tiles = list(range(2, 2 + 16))
                with ExitStack() as c2:
                    wr, _ = self.TB(c2, "r_wr", [128, NEXP, D], F32)
                    b_wr = [Buf(f"wr{ex}") for ex in range(NEXP)]
                    rs, b_rs = self.TB(c2, "r_rs", [128, 48], F32)
                    junk, b_junk = self.TB(c2, "r_junk", [128, D], F32)
                    for ex in range(NEXP):
                        S.dma(S.sp, lambda e: e.dma_start(out=wr[:, ex, :], in_=self.dram["moe_router_t"][layer // 2, ex:ex + 1, :].partition_broadcast(128)),
                              [self.bufs["moe_router_t"]], [b_wr[ex]])
                    self.stage_norm(layer, 1, "hbuf", aT, aT_bufs, half_tiles, router=(gates, b_gates, wr, b_wr, rs, b_rs, junk, b_junk),
                                    gather=(tix, b_tix))
                self.stage_glu(layer, aT, aT_bufs, self.glu_passes(layer), 2, gates=(gates, b_gates), ntile=16)
                self.stage_final(layer, last, gather=(tix, b_tix))
                return
        self.stage_final(layer, last)

    def build(self):
        nc, S = self.nc, self.S
        self.declare_io()
        c = self.stack
        self.psall = c.enter_context(nc.psum_tensor("psall", [128, 4096], F32))
        self.ps = [self.psall[:, i * 512:(i + 1) * 512] for i in range(8)]
        self.psb = [Buf(f"ps{i}") for i in range(8)]
        aT = self.sb(c, "aT", [128, KC, T], BF16)
        aT_bufs = [Buf(f"aT{i}") for i in range(NT)]
        self.declare_proj()
        self.declare_scan()
        self.declare_na()
        self.declare_tail()
        if self.upto == "l0merge":
            self.run_layer(0, aT, aT_bufs, stop_after="merge")
        elif self.upto == "l0":
            self.run_layer(0, aT, aT_bufs)
        else:
            for layer in range(DEPTH):
                self.stage_mod(layer)
            self.mods_done = True
            for layer in range(DEPTH):
                self.run_layer(layer, aT, aT_bufs)
        if "hbuf" in self.debug:
            o = self.dout("dbg_hbuf", [T, D])
            S.dma(S.sp, lambda e: e.dma_start(out=o[:, :], in_=self.dram["hbuf"][:, :]), self.h_bufs + [self.bufs["hbuf"]], [self.bufs["dbg_hbuf"]])
        return self.finish()

    def finish(self):
        S = self.S
        if "modv" in self.debug:
            o = self.dout("dbg_modv", [DEPTH, 2, 8, D])
            S.dma(S.sp, lambda e: e.dma_start(out=o[:, :, :, :], in_=self.dram["modv"][:, :, :, :]),
                  [self.bufs["modv"]], [self.bufs["dbg_modv"]])
        S.barrier()
        self.stack.close()
        return self.nc


def host_consts():
    m = {}
    sidx = np.arange(128)[:, None]; tidx = np.arange(128)[None, :]
    same = (sidx // 64) == (tidx // 64)
    ref = (tidx // 64) * 64 + 31
    ref2 = (tidx // 64) * 64 + 32
    mxf = same * ((sidx <= tidx).astype(np.float32) - (sidx <= ref).astype(np.float32))
    mxb = same * ((sidx < ref2).astype(np.float32) - (sidx < tidx).astype(np.float32))
    m["mx"] = np.stack([mxf, mxb]).astype(np.float32)
    sel = np.zeros((2, 128, 6), np.float32)
    s1 = np.arange(128)
    for cix in range(2):
        inc = (s1 // 64) == cix
        first = inc & (s1 % 64 <= 31)
        second = inc & (s1 % 64 >= 32)
        sel[0, :, cix * 3 + 0] = first; sel[0, :, cix * 3 + 1] = inc; sel[0, :, cix * 3 + 2] = second
        sel[1, :, cix * 3 + 0] = second; sel[1, :, cix * 3 + 1] = inc; sel[1, :, cix * 3 + 2] = first
    m["sel"] = sel
    m["selrep"] = np.ascontiguousarray(np.broadcast_to(sel.transpose(0, 2, 1)[:, :, :, None], (2, 6, 128, 128))).astype(np.float32)
    am = np.stack([same & ((sidx % 64) <= (tidx % 64)), same & ((sidx % 64) >= (tidx % 64))]).astype(np.float32)
    m["amask"] = np.ascontiguousarray(np.broadcast_to(am[:, :, None, :], (2, 128, 4, 128))).astype(np.float32)
    tt = np.arange(SEQ)
    inv_freq = (10000.0 ** (-np.arange(32, dtype=np.float32) / 32)).astype(np.float32)
    ar = (tt // 64).astype(np.float32)[:, None] * inv_freq
    ac = (tt % 64).astype(np.float32)[:, None] * inv_freq
    m["rope"] = np.stack([np.cos(ar), np.sin(ar), np.cos(ac), np.sin(ac)], axis=1).astype(np.float32)
    return m


_NA_IDX = None


def na_bias_tables(rpb):
    global _NA_IDX
    if _NA_IDX is None:
        idx = np.zeros((5, 128, 5, 128, 2), np.int64)
        msk = np.zeros((5, 128, 5, 128), bool)
        for cls, qb in enumerate((5, 0, 1, 30, 31)):
            R0 = min(max(2 * qb - 4, 0), 54)
            q = np.arange(128); r = 2 * qb + q // 64; qc = q % 64
            r0 = np.clip(r - 4, 0, 56); ws = np.clip(qc - 8, 0, 48)
            for j in range(5):
                k = np.arange(128); kr = R0 + 2 * j + k // 64; kc = k % 64
                inw = ((kr[:, None] >= r0[None, :]) & (kr[:, None] < r0[None, :] + 8) &
                       (kc[:, None] >= ws[None, :]) & (kc[:, None] < ws[None, :] + 16))
                drr = np.clip(kr[:, None] - r[None, :] + 7, 0, 14)
                dcc = np.clip(kc[:, None] - qc[None, :] + 15, 0, 30)
                idx[cls, :, j, :, 0] = drr; idx[cls, :, j, :, 1] = dcc; msk[cls, :, j, :] = inw
        _NA_IDX = (idx, msk)
    idx, msk = _NA_IDX
    g = rpb[:, :, idx[..., 0], idx[..., 1]]
    g = np.where(msk[None, None], g, np.float32(-1e30)).astype(np.float32)
    return np.ascontiguousarray(g.transpose(0, 2, 1, 3, 4, 5))


def host_inputs(inputs, b, g=0):
    x, c, ctx, c_ctx = inputs["x"], inputs["c"], inputs["ctx"], inputs["c_ctx"]
    m = {}
    m["xin"] = np.ascontiguousarray(np.concatenate([ctx[b], x[b]], axis=0))
    cv = np.stack([c[b].reshape(KC, 128).T, c_ctx.reshape(KC, 128).T], axis=-1)
    m["cvec"] = np.ascontiguousarray(cv.astype(np.float32))
    m["ident"] = np.eye(128, dtype=np.float32)
    m["tokidx"] = np.ascontiguousarray(((2 + 16 * g + np.arange(16))[None, :] * 128 + np.arange(128)[:, None]).astype(np.uint32))
    m.update(host_consts())
    m["nabias"] = na_bias_tables(inputs["na_rpb"])
    m["ml_gate_b"] = np.ascontiguousarray(inputs["ml_gate_b"].reshape(DEPTH, 16))
    for k in ("hg_lb", "hg_norm_w", "ml_norm_w"):
        m[k] = np.ascontiguousarray(inputs[k])
    m["moe_router_t"] = np.ascontiguousarray(inputs["moe_router"].transpose(0, 2, 1))
    for k in ("w_branch", "w_out", "ffn_w_up", "ffn_w_down", "moe_w_up", "moe_w_down", "final_norm_w"):
        m[k] = np.ascontiguousarray(inputs[k])
    for k in ("mod_w", "mod_b", "norm1_w", "norm2_w", "w_in"):
        m[k] = np.ascontiguousarray(inputs[k])
    return m


_PROG = None


def kernel(**inputs):
    global _PROG
    inputs = {k: np.asarray(v) for k, v in inputs.items()}
    if _PROG is None:
        _PROG = Prog(upto="all")
        _PROG.build()
    P = _PROG
    names = [n for n in P.dram if n in P.in_names]
    shared = None
    maps = []
    for core in range(8):
        m = host_inputs(inputs, core % 4, core // 4)
        maps.append({k: m[k] for k in names})
    res = run_bass_kernel_spmd(P.nc, maps, core_ids=list(range(8)))
    out = np.empty((4, SEQ, D), np.float32)
    for core in range(8):
        b, g = core % 4, core // 4
        out[b, g * (SEQ // 2):(g + 1) * (SEQ // 2)] = np.asarray(res.results[core]["out"])
    return out
```

```python
import numpy as np
import concourse.bass as bass
import concourse.mybir as mybir
from concourse.bass_utils import run_bass_kernel_spmd
from contextlib import ExitStack

F32 = mybir.dt.float32
BF16 = mybir.dt.bfloat16
I32 = mybir.dt.int32
U32 = mybir.dt.uint32
AF = mybir.ActivationFunctionType
ALU = mybir.AluOpType
AX = mybir.AxisListType

D = 1024
KC = 8
CTX = 256
SEQ = 4096
T = CTX + SEQ
NT = T // 128
DEPTH = 2
IN_W = 9232
FFN = 2816
NEXP = 8
EDIM = 3584
EPS = 1e-6
C_NAQ, C_NAK, C_NAV = 0, 512, 1024
C_HGQ, C_HGF, C_HGB, C_HGI, C_HGG = 1536, 2048, 2560, 3072, 3584
C_MLQ, C_MLK, C_MLV, C_MLO, C_MLG, C_BG = 4096, 4608, 5120, 5632, 6144, 6160


def _interleave(*gens):
    gens = [iter(g) for g in gens]
    done = [False] * len(gens)
    while not all(done):
        for k, g in enumerate(gens):
            if not done[k]:
                try:
                    next(g)
                except StopIteration:
                    done[k] = True


class Tok:
    __slots__ = ("sem", "val", "clk")

    def __init__(self, sem, val, clk):
        self.sem, self.val, self.clk = sem, val, clk


class Buf:
    __slots__ = ("name", "w", "rs")

    def __init__(self, name):
        self.name, self.w, self.rs = name, None, {}


class Eng:
    def __init__(self, name, h, sem):
        self.name, self.h, self.sem = name, h, sem
        self.count = 0
        self.seen = {}
        self.dma_sems = []
        self.dma_vals = []
        self.dma_rr = 0


class Sched:
    def __init__(self, nc, stack, n_dma_sems=12):
        self.nc = nc
        mk = lambda n: stack.enter_context(nc.semaphore(n))
        self.pe = Eng("pe", nc.tensor, mk("s_pe"))
        self.act = Eng("act", nc.scalar, mk("s_act"))
        self.dve = Eng("dve", nc.vector, mk("s_dve"))
        self.pool = Eng("pool", nc.gpsimd, mk("s_pool"))
        self.sp = Eng("sp", nc.sync, mk("s_sp"))
        self.engs = [self.pe, self.act, self.dve, self.pool, self.sp]
        for q in (self.sp, self.pool):
            for i in range(n_dma_sems):
                q.dma_sems.append(mk(f"d_{q.name}{i}"))
                q.dma_vals.append(0)
        self.n_inst = 0

    def _need(self, eng, tok, kind):
        if tok is None:
            return
        if tok.sem is eng.sem:
            if eng is self.pe:
                return
        k = id(tok.sem)
        if eng.seen.get(k, 0) >= tok.val:
            return
        eng.h.wait_ge(tok.sem, tok.val)
        self.n_inst += 1
        seen = eng.seen
        for kk, vv in tok.clk.items():
            if seen.get(kk, 0) < vv:
                seen[kk] = vv
        seen[k] = tok.val

    def _deps(self, eng, reads, writes):
        for b in reads:
            self._need(eng, b.w, "raw")
        for b in writes:
            self._need(eng, b.w, "waw")
            for r in b.rs.values():
                self._need(eng, r, "war")

    def _commit(self, tok, reads, writes):
        k = id(tok.sem)
        for b in reads:
            o = b.rs.get(k)
            if o is None or o.val < tok.val:
                b.rs[k] = tok
        for b in writes:
            b.w = tok
            b.rs = {}

    def op(self, eng, fn, reads=(), writes=(), inc=True):
        self._deps(eng, reads, writes)
        ins = fn(eng.h)
        self.n_inst += 1
        if inc:
            ins.then_inc(eng.sem, 1)
            eng.count += 1
            val = eng.count
        else:
            val = eng.count + 1
        clk = dict(eng.seen)
        tok = Tok(eng.sem, val, clk)
        self._commit(tok, reads, writes)
        return tok

    def dma(self, q, fn, reads=(), writes=()):
        i = q.dma_rr
        q.dma_rr = (i + 1) % len(q.dma_sems)
        sem = q.dma_sems[i]
        prev = q.dma_vals[i]
        k = id(sem)
        if prev and q.seen.get(k, 0) < prev:
            q.h.wait_ge(sem, prev)
            q.seen[k] = prev
        self._deps(q, reads, writes)
        ins = fn(q.h)
        ins.then_inc(sem, 16)
        self.n_inst += 1
        q.dma_vals[i] = prev + 16
        tok = Tok(sem, prev + 16, dict(q.seen))
        self._commit(tok, reads, writes)
        return tok

    def barrier(self):
        for e in self.engs:
            for f in self.engs:
                if f is not e and f.count and e.seen.get(id(f.sem), 0) < f.count:
                    e.h.wait_ge(f.sem, f.count)
                    e.seen[id(f.sem)] = f.count
            for q in (self.sp, self.pool):
                for sem, v in zip(q.dma_sems, q.dma_vals):
                    if v and e.seen.get(id(sem), 0) < v:
                        e.h.wait_ge(sem, v)
                        e.seen[id(sem)] = v


class Prog:
    def __init__(self, upto="all", debug=()):
        self.upto = upto
        self.debug = set(debug)
        self.nc = nc = bass.Bass("TRN2", target_bir_lowering=False)
        self.stack = ExitStack()
        self.S = Sched(nc, self.stack)
        self.dram = {}
        self.bufs = {}

    def din(self, name, shape, dt=F32):
        self.in_names = getattr(self, "in_names", set()) | {name}
        t = self.nc.dram_tensor(name, list(shape), dt, kind="ExternalInput").ap()
        self.dram[name] = t
        self.bufs[name] = Buf(name)
        return t

    def dout(self, name, shape, dt=F32):
        t = self.nc.dram_tensor(name, list(shape), dt, kind="ExternalOutput").ap()
        self.dram[name] = t
        self.bufs[name] = Buf(name)
        return t

    def dscr(self, name, shape, dt=F32):
        t = self.nc.dram_tensor(name, list(shape), dt).ap()
        self.dram[name] = t
        self.bufs[name] = Buf(name)
        return t

    def sb(self, ctx, name, shape, dt):
        self.uid = getattr(self, "uid", 0) + 1
        return ctx.enter_context(self.nc.sbuf_tensor(f"{name}_u{self.uid}", list(shape), dt))

    def declare_io(self):
        self.din("xin", [T, D])
        self.din("cvec", [128, KC, 2])
        self.din("ident", [128, 128])
        self.din("mod_w", [DEPTH, D, 6 * D])
        self.din("mod_b", [DEPTH, 6 * D])
        self.din("norm1_w", [DEPTH, D])
        self.din("norm2_w", [DEPTH, D])
        self.din("w_in", [DEPTH, D, IN_W])
        self.dscr("modv", [DEPTH, 2, 8, D])
        self.dscr("hbuf", [T, D])

    def stage_mod(self, layer):
        nc, S = self.nc, self.S
        B = self.bufs
        with ExitStack() as c:
            cv = self.sb(c, "m_cv", [128, KC, 2], F32)
            cs = self.sb(c, "m_cs", [128, KC, 2], BF16)
            sg = self.sb(c, "m_sg", [128, KC, 2], F32)
            wt = [self.sb(c, f"m_wt{i}", [128, KC, 512], BF16) for i in range(2)]
            res = self.sb(c, "m_res", [2, 8, D], F32)
            mb = self.sb(c, "m_mb", [2, 6 * D], F32)
            nw = self.sb(c, "m_nw", [2, 2, D], F32)
            b_cv, b_cs, b_sg, b_res, b_mb, b_nw = (Buf(n) for n in ("cv", "cs", "sg", "res", "mb", "nw"))
            b_wt = [Buf("wt0"), Buf("wt1")]
            ps = self.ps
            S.dma(S.sp, lambda e: e.dma_start(out=cv[:], in_=self.dram["cvec"][:, :, :]), [B["cvec"]], [b_cv])
            for r in range(2):
                S.dma(S.sp, lambda e, r=r: e.dma_start(out=mb[r:r + 1, :], in_=self.dram["mod_b"][layer:layer + 1, :]),
                      [B["mod_b"]], [b_mb])
                S.dma(S.sp, lambda e, r=r: e.dma_start(out=nw[r:r + 1, 0, :], in_=self.dram["norm1_w"][layer:layer + 1, :]),
                      [B["norm1_w"]], [b_nw])
                S.dma(S.sp, lambda e, r=r: e.dma_start(out=nw[r:r + 1, 1, :], in_=self.dram["norm2_w"][layer:layer + 1, :]),
                      [B["norm2_w"]], [b_nw])
            S.op(S.act, lambda e: e.activation(out=sg[:], in_=cv[:], func=AF.Sigmoid), [b_cv], [b_sg])
            S.op(S.dve, lambda e: e.tensor_tensor(out=cs[:], in0=cv[:], in1=sg[:], op=ALU.mult), [b_cv, b_sg], [b_cs])
            mw = self.dram["mod_w"]
            for j in range(12):
                w = wt[j % 2]
                S.dma(S.pool, lambda e, j=j, w=w: e.dma_start(
                    out=w[:], in_=mw[layer, :, j * 512:(j + 1) * 512].rearrange("(kc p) n -> p kc n", p=128)),
                    [B["mod_w"]], [b_wt[j % 2]])
                pb = self.psb[j % 2]
                for k in range(KC):
                    S.op(S.pe, lambda e, k=k, w=w, j=j: e.matmul(ps[j % 2][0:2, :], lhsT=cs[:, k, :], rhs=w[:, k, :],
                                                              start=(k == 0), stop=(k == KC - 1)),
                         [b_cs, b_wt[j % 2]], [pb], inc=(k == KC - 1))
                g, o = divmod(j * 512, D)
                S.op(S.dve, lambda e, j=j, g=g, o=o: e.tensor_tensor(
                    out=res[:, g, o:o + 512], in0=ps[j % 2][0:2, :], in1=mb[:, j * 512:(j + 1) * 512], op=ALU.add),
                    [pb, b_mb], [b_res])
            for r, (sc, wi) in enumerate(((1, 0), (4, 1))):
                S.op(S.dve, lambda e, r=r, sc=sc, wi=wi: e.scalar_tensor_tensor(
                    out=res[:, 6 + r, :], in0=res[:, sc, :], scalar=1.0, in1=nw[:, wi, :], op0=ALU.add, op1=ALU.mult),
                    [b_res, b_nw], [b_res])
            S.dma(S.sp, lambda e: e.dma_start(out=self.dram["modv"][layer], in_=res[:]), [b_res], [B["modv"]])
            S.barrier()

    def load_row_bcast(self, dst, layer, r, row, dst_buf):
        src = self.dram["modv"][layer, r, row:row + 1, :].partition_broadcast(128)
        self.S.dma(self.S.sp, lambda e: e.dma_start(out=dst, in_=src), [self.bufs["modv"]], [dst_buf])

    def stage_norm(self, layer, which, src_name, aT, aT_bufs, tiles, router=None, gather=None):
        nc, S, B = self.nc, self.S, self.bufs
        grow, srow = ((6, 0), (7, 3))[which]
        src = self.dram[src_name]
        with ExitStack() as c:
            gv = [self.sb(c, f"n_gv{r}", [128, D], F32) for r in range(2)]
            sv = [self.sb(c, f"n_sv{r}", [128, D], F32) for r in range(2)]
            b_gv = [Buf("gv0"), Buf("gv1")]
            b_sv = [Buf("sv0"), Buf("sv1")]
            idb = self.sb(c, "n_id", [128, 128], BF16)
            b_id = Buf("id")
            S.dma(S.pool, lambda e: e.dma_start(out=idb[:], in_=self.dram["ident"][:, :]), [B["ident"]], [b_id])
            for r in range(2):
                self.load_row_bcast(gv[r][:], layer, r, grow, b_gv[r])
                self.load_row_bcast(sv[r][:], layer, r, srow, b_sv[r])
            NB = 3
            ht = [self.sb(c, f"n_h{i}", [128, D], F32) for i in range(NB)]
            b_h = [Buf(f"h{i}") for i in range(NB)]
            sq = self.sb(c, "n_sq", [128, D], BF16)
            b_sq = Buf("sq")
            st = [self.sb(c, f"n_st{i}", [128, 4], F32) for i in range(NB)]
            b_st = [Buf(f"st{i}") for i in range(NB)]
            t1 = [self.sb(c, f"n_t1{i}", [128, D], F32) for i in range(2)]
            b_t1 = [Buf("t10"), Buf("t11")]
            ab = [self.sb(c, f"n_ab{i}", [128, D], BF16) for i in range(2)]
            b_ab = [Buf("ab0"), Buf("ab1")]

            def load(i):
                ti = tiles[i]
                if gather is None:
                    S.dma(S.sp, lambda e: e.dma_start(out=ht[i % NB][:], in_=src[ti * 128:(ti + 1) * 128, :]),
                          [B[src_name]] + ([self.h_bufs[ti]] if src_name == "hbuf" else []), [b_h[i % NB]])
                else:
                    S.dma(S.pool, lambda e: e.indirect_dma_start(out=ht[i % NB][:], out_offset=None, in_=src[:, :],
                                                                 in_offset=bass.IndirectOffsetOnAxis(ap=gather[0][:, ti - 2:ti - 1], axis=0)),
                          [B[src_name], gather[1]] + self.h_bufs, [b_h[i % NB]])

            def statsA(i, ti):
                    r = 1 if ti < CTX // 128 else 0
                    h, s_, bh, bs = ht[i % NB], st[i % NB], b_h[i % NB], b_st[i % NB]
                    S.op(S.act, lambda e: e.activation(out=sq[:], in_=h[:], func=AF.Square, accum_out=s_[:, 0:1]),
                         [bh], [b_sq, bs])
                    yield
                    S.op(S.dve, lambda e: e.tensor_scalar(out=s_[:, 1:2], in0=s_[:, 0:1], scalar1=1.0 / D, scalar2=EPS,
                                                          op0=ALU.mult, op1=ALU.add), [bs], [bs])
                    yield
                    S.op(S.act, lambda e: e.activation(out=s_[:, 2:3], in_=s_[:, 1:2], func=AF.Ln), [bs], [bs])
                    yield
                    S.op(S.act, lambda e: e.activation(out=s_[:, 3:4], in_=s_[:, 2:3], func=AF.Exp, scale=-0.5), [bs], [bs])
                    yield

            def applyB(i, ti):
                    r = 1 if ti < CTX // 128 else 0
                    h, s_, bh, bs = ht[i % NB], st[i % NB], b_h[i % NB], b_st[i % NB]
                    tt, bt = t1[i % 2], b_t1[i % 2]
                    a_, ba = ab[i % 2], b_ab[i % 2]
                    S.op(S.dve, lambda e: e.scalar_tensor_tensor(out=tt[:], in0=h[:], scalar=s_[:, 3:4], in1=gv[r][:],
                                                                 op0=ALU.mult, op1=ALU.mult), [bh, bs, b_gv[r]], [bt])
                    yield
                    if router is None:
                        S.op(S.pool, lambda e: e.tensor_tensor(out=a_[:], in0=tt[:], in1=sv[r][:], op=ALU.add),
                             [bt, b_sv[r]], [ba])
                        yield
                    else:
                        gates, b_gates, wr, b_wr, rs, b_rs, junk, b_junk = router
                        S.op(S.pool, lambda e: e.tensor_tensor(out=tt[:], in0=tt[:], in1=sv[r][:], op=ALU.add), [bt, b_sv[r]], [bt])
                        yield
                        S.op(S.act, lambda e: e.copy(out=a_[:], in_=tt[:]), [bt], [ba])
                        yield
                        for ex in range(NEXP):
                            S.op(S.dve, lambda e: e.scalar_tensor_tensor(out=junk[:], in0=tt[:], scalar=1.0, in1=wr[:, ex, :], op0=ALU.mult, op1=ALU.mult,
                                                                         accum_out=rs[:, ex:ex + 1]), [bt, b_wr[ex]], [b_junk, b_rs])
                            yield
                        lg, mk1, lg2, mk2 = rs[:, 0:8], rs[:, 8:16], rs[:, 16:24], rs[:, 24:32]
                        S.op(S.dve, lambda e: e.tensor_reduce(out=rs[:, 32:33], in_=lg, op=ALU.max, axis=AX.X), [b_rs], [b_rs])
                        yield
                        S.op(S.dve, lambda e: e.tensor_scalar(out=mk1, in0=lg, scalar1=rs[:, 32:33], scalar2=None, op0=ALU.is_equal), [b_rs], [b_rs])
                        yield
                        S.op(S.dve, lambda e: e.scalar_tensor_tensor(out=lg2, in0=mk1, scalar=-1e30, in1=lg, op0=ALU.mult, op1=ALU.add), [b_rs], [b_rs])
                        yield
                        S.op(S.dve, lambda e: e.tensor_reduce(out=rs[:, 33:34], in_=lg2, op=ALU.max, axis=AX.X), [b_rs], [b_rs])
                        yield
                        S.op(S.dve, lambda e: e.tensor_scalar(out=mk2, in0=lg2, scalar1=rs[:, 33:34], scalar2=None, op0=ALU.is_equal), [b_rs], [b_rs])
                        yield
                        S.op(S.dve, lambda e: e.tensor_tensor(out=rs[:, 34:35], in0=rs[:, 33:34], in1=rs[:, 32:33], op=ALU.subtract), [b_rs], [b_rs])
                        yield
                        S.op(S.act, lambda e: e.activation(out=rs[:, 35:36], in_=rs[:, 34:35], func=AF.Exp), [b_rs], [b_rs])
                        yield
                        S.op(S.dve, lambda e: e.tensor_scalar(out=rs[:, 36:37], in0=rs[:, 35:36], scalar1=1.0, scalar2=None, op0=ALU.add), [b_rs], [b_rs])
                        yield
                        S.op(S.dve, lambda e: e.reciprocal(out=rs[:, 37:38], in_=rs[:, 36:37]), [b_rs], [b_rs])
                        yield
                        S.op(S.dve, lambda e: e.tensor_tensor(out=rs[:, 38:39], in0=rs[:, 35:36], in1=rs[:, 37:38], op=ALU.mult), [b_rs], [b_rs])
                        yield
                        S.op(S.dve, lambda e: e.tensor_scalar(out=rs[:, 40:48], in0=mk1, scalar1=rs[:, 37:38], scalar2=None, op0=ALU.mult), [b_rs], [b_rs])
                        yield
                        S.op(S.dve, lambda e: e.scalar_tensor_tensor(out=gates[:, ti, :], in0=mk2, scalar=rs[:, 38:39], in1=rs[:, 40:48], op0=ALU.mult, op1=ALU.add),
                             [b_rs], [b_gates])
                        yield
                    pb = self.psb[i % 2]
                    pt = self.ps[i % 2][:].bitcast(BF16)
                    for k in range(KC):
                        S.op(S.pe, lambda e, k=k: e.transpose(pt[:, k * 128:(k + 1) * 128], a_[:, k * 128:(k + 1) * 128], idb[:]),
                             [ba, b_id], [pb], inc=(k == KC - 1))
                        yield
                    eng = S.act if i % 2 == 0 else S.dve
                    dst = aT[:, :, ti * 128:(ti + 1) * 128]
                    srcp = pt.rearrange("p (k t) -> p k t", k=KC)
                    if eng is S.act:
                        S.op(eng, lambda e: e.copy(out=dst, in_=srcp), [pb], [aT_bufs[ti]])
                        yield
                    else:
                        S.op(eng, lambda e: e.tensor_copy(out=dst, in_=srcp), [pb], [aT_bufs[ti]])
                        yield

            for i in range(min(2, len(tiles))):
                load(i)
            for _ in statsA(0, tiles[0]):
                pass
            for i, ti in enumerate(tiles):
                nxt = statsA(i + 1, tiles[i + 1]) if i + 1 < len(tiles) else iter(())
                _interleave(nxt, applyB(i, ti))
                if i + 2 < len(tiles):
                    load(i + 2)
            S.barrier()


    def declare_proj(self):
        self.dscr("p_naqk", [1024, T], BF16)
        for n in ("p_nav", "p_hgq", "p_hgi", "p_hgg", "p_mlq", "p_mlk", "p_mlv", "p_mlo"):
            self.dscr(n, [T, 512], BF16)
        self.dscr("p_hgf", [T, 512], F32)
        self.dscr("p_hgb", [T, 512], F32)
        self.dscr("p_mlg", [T, 16], F32)

    def stage_inproj(self, layer, aT, aT_bufs):
        nc, S, B = self.nc, self.S, self.bufs
        w_in = self.dram["w_in"]
        tm_blocks = [("p_nav", C_NAV, BF16), ("p_hgq", C_HGQ, BF16), ("p_hgf", C_HGF, F32), ("p_hgb", C_HGB, F32),
                     ("p_hgi", C_HGI, BF16), ("p_hgg", C_HGG, BF16), ("p_mlq", C_MLQ, BF16), ("p_mlk", C_MLK, BF16),
                     ("p_mlv", C_MLV, BF16), ("p_mlo", C_MLO, BF16)]
        with ExitStack() as c:
            wt = [self.sb(c, f"p_wt{i}", [128, KC, 512], BF16) for i in range(2)]
            b_wt = [Buf("pwt0"), Buf("pwt1")]
            wg = self.sb(c, "p_wg", [128, KC, 16], BF16)
            b_wg = Buf("pwg")
            stg_b = [self.sb(c, f"p_sb{i}", [128, 4, 512], BF16) for i in range(2)]
            stg_f = [self.sb(c, f"p_sf{i}", [128, 4, 512], F32) for i in range(2)]
            b_sb = [Buf("psb0"), Buf("psb1")]
            b_sf = [Buf("psf0"), Buf("psf1")]
            stg_g = self.sb(c, "p_sg", [128, NT, 16], F32)
            b_sg = Buf("psg")
            cnt = {"w": 0, "ev": 0, "ps": 0, "st": 0}

            def load_w(col0, width=512):
                i = cnt["w"] % 2
                cnt["w"] += 1
                S.dma(S.pool, lambda e: e.dma_start(
                    out=wt[i][:, :, 0:width],
                    in_=w_in[layer, :, col0:col0 + width].rearrange("(kc p) n -> p kc n", p=128)),
                    [B["w_in"]], [b_wt[i]])
                return wt[i], b_wt[i]

            def evac(dst, src, rd, wr):
                if cnt["ev"] % 2 == 0:
                    S.op(S.act, lambda e: e.copy(out=dst, in_=src), rd, wr)
                else:
                    S.op(S.dve, lambda e: e.tensor_copy(out=dst, in_=src), rd, wr)
                cnt["ev"] += 1

            def next_ps():
                i = cnt["ps"] % 4
                cnt["ps"] += 1
                return self.ps[i], self.psb[i]

            blocks = [("fm", "q", C_NAQ), ("fm", "k", C_NAK)] + [("tm",) + b for b in tm_blocks]
            pending = load_w(blocks[0][2])
            for bi, blk in enumerate(blocks):
                w, bw = pending
                if bi + 1 < len(blocks):
                    nb = blocks[bi + 1]
                    pending = load_w(nb[2])
                if blk[0] == "fm":
                    row0 = 0 if blk[1] == "q" else 512
                    for m in range(4):
                        for tb in range(0, T, 512):
                            n = min(512, T - tb)
                            ps, pb = next_ps()
                            tiles = range(tb // 128, (tb + n) // 128)
                            for k in range(KC):
                                S.op(S.pe, lambda e: e.matmul(ps[:, 0:n], lhsT=w[:, k, m * 128:(m + 1) * 128],
                                                              rhs=aT[:, k, tb:tb + n], start=(k == 0), stop=(k == KC - 1)),
                                     [bw] + [aT_bufs[t] for t in tiles], [pb], inc=(k == KC - 1))
                            si = cnt["st"] % 2
                            cnt["st"] += 1
                            evac(stg_b[si][:, 0, 0:n], ps[:, 0:n], [pb], [b_sb[si]])
                            S.dma(S.sp, lambda e: e.dma_start(
                                out=self.dram["p_naqk"][row0 + m * 128:row0 + (m + 1) * 128, tb:tb + n],
                                in_=stg_b[si][:, 0, 0:n]), [b_sb[si]], [B["p_naqk"]])
                else:
                    _, name, col0, dt = blk
                    stg, bst = (stg_b, b_sb) if dt == BF16 else (stg_f, b_sf)
                    for g0 in range(0, NT, 4):
                        gn = min(4, NT - g0)
                        si = cnt["st"] % 2
                        cnt["st"] += 1
                        for j in range(gn):
                            ti = g0 + j
                            ps, pb = next_ps()
                            for k in range(KC):
                                S.op(S.pe, lambda e: e.matmul(ps[:, :], lhsT=aT[:, k, ti * 128:(ti + 1) * 128],
                                                              rhs=w[:, k, :], start=(k == 0), stop=(k == KC - 1)),
                                     [bw, aT_bufs[ti]], [pb], inc=(k == KC - 1))
                            evac(stg[si][:, j, :], ps[:, :], [pb], [bst[si]])
                        S.dma(S.sp, lambda e: e.dma_start(
                            out=self.dram[name][g0 * 128:(g0 + gn) * 128, :].rearrange("(j p) n -> p j n", p=128),
                            in_=stg[si][:, 0:gn, :]), [bst[si]], [B[name]])
            S.dma(S.pool, lambda e: e.dma_start(
                out=wg[:], in_=w_in[layer, :, C_MLG:C_MLG + 16].rearrange("(kc p) n -> p kc n", p=128)),
                [B["w_in"]], [b_wg])
            for ti in range(NT):
                ps, pb = next_ps()
                for k in range(KC):
                    S.op(S.pe, lambda e: e.matmul(ps[:, 0:16], lhsT=aT[:, k, ti * 128:(ti + 1) * 128],
                                                  rhs=wg[:, k, :], start=(k == 0), stop=(k == KC - 1)),
                         [b_wg, aT_bufs[ti]], [pb], inc=(k == KC - 1))
                evac(stg_g[:, ti, :], ps[:, 0:16], [pb], [b_sg])
            S.dma(S.sp, lambda e: e.dma_start(out=self.dram["p_mlg"].rearrange("(j p) n -> p j n", p=128), in_=stg_g[:]),
                  [b_sg], [B["p_mlg"]])
            S.barrier()


    def TB(self, ctx, name, shape, dt):
        return self.sb(ctx, name, shape, dt), Buf(name)

    def declare_scan(self):
        self.din("mx", [2, 128, 128])
        self.din("sel", [2, 128, 6])
        self.din("selrep", [2, 6, 128, 128])
        self.din("amask", [2, 128, 4, 128])
        self.din("rope", [SEQ, 4, 32])
        self.din("hg_lb", [DEPTH, 2, 512])
        self.din("hg_norm_w", [DEPTH, 512])
        self.din("ml_norm_w", [DEPTH, 512])
        self.din("ml_gate_b", [DEPTH, 16])
        self.dscr("of_hg", [T, 512], F32)
        self.dscr("of_ml", [T, 512], F32)
        self.dscr("brT", [3, 512, T], BF16)

    def stage_scan(self, layer, kind, last):
        nc, S, B, ps, psb = self.nc, self.S, self.bufs, self.ps, self.psb
        H = 4
        DV = 128 if kind == "hg" else 129
        dr = self.dram
        of_name = "of_hg" if kind == "hg" else "of_ml"
        br_idx = 1 if kind == "hg" else 2
        with ExitStack() as c:
            idb, b_id = self.TB(c, "s_id", [128, 128], BF16)
            S.dma(S.pool, lambda e: e.dma_start(out=idb[:], in_=dr["ident"][:, :]), [B["ident"]], [b_id])
            mx, b_mx = self.TB(c, "s_mx", [128, 2, 128], F32)
            S.dma(S.sp, lambda e: e.dma_start(out=mx[:], in_=dr["mx"].rearrange("d s t -> s d t")), [B["mx"]], [b_mx])
            am, b_am = self.TB(c, "s_am", [128, 2, 4 * 128], F32)
            S.dma(S.sp, lambda e: e.dma_start(out=am[:], in_=dr["amask"].rearrange("d p h t -> p d (h t)")), [B["amask"]], [b_am])
            nwn = "hg_norm_w" if kind == "hg" else "ml_norm_w"
            nw, b_nw = self.TB(c, "s_nw", [128, 512], F32)
            S.dma(S.sp, lambda e: e.dma_start(out=nw[:], in_=dr[nwn][layer:layer + 1, :].partition_broadcast(128)), [B[nwn]], [b_nw])
            if kind == "hg":
                selt, b_sel = self.TB(c, "s_sel", [128, 2, 6], F32)
                S.dma(S.sp, lambda e: e.dma_start(out=selt[:], in_=dr["sel"].rearrange("d s j -> s d j")), [B["sel"]], [b_sel])
                lb, b_lb = self.TB(c, "s_lb", [128, 2, 512], F32)
                oml, b_oml = self.TB(c, "s_oml", [128, 2, 512], F32)
                if layer == 0:
                    S.op(S.dve, lambda e: e.memset(lb[:], 0.0), [], [b_lb])
                    S.op(S.dve, lambda e: e.memset(oml[:], 1.0), [], [b_oml])
                else:
                    l0, b_l0 = self.TB(c, "s_l0", [128, 2, 512], F32)
                    l1, b_l1 = self.TB(c, "s_l1", [128, 2, 512], F32)
                    for dd in range(2):
                        S.dma(S.sp, lambda e: e.dma_start(out=l0[:, dd, :], in_=dr["hg_lb"][0, dd:dd + 1, :].partition_broadcast(128)), [B["hg_lb"]], [b_l0])
                        S.dma(S.sp, lambda e: e.dma_start(out=l1[:, dd, :], in_=dr["hg_lb"][1, dd:dd + 1, :].partition_broadcast(128)), [B["hg_lb"]], [b_l1])
                    S.op(S.dve, lambda e: e.tensor_tensor(out=l0[:], in0=l0[:], in1=l1[:], op=ALU.subtract), [b_l0, b_l1], [b_l0])
                    S.op(S.act, lambda e: e.activation(out=l1[:], in_=l0[:], func=AF.Exp), [b_l0], [b_l1])
                    S.op(S.dve, lambda e: e.tensor_scalar(out=l0[:], in0=l1[:], scalar1=1.0, scalar2=None, op0=ALU.add), [b_l1], [b_l0])
                    S.op(S.dve, lambda e: e.reciprocal(out=lb[:], in_=l0[:]), [b_l0], [b_lb])
                    S.op(S.dve, lambda e: e.tensor_tensor(out=oml[:], in0=l1[:], in1=lb[:], op=ALU.mult), [b_l1, b_lb], [b_oml])
            else:
                selr, b_selr = self.TB(c, "s_selr", [128, 2, 6, 128], F32)
                S.dma(S.sp, lambda e: e.dma_start(out=selr[:], in_=dr["selrep"].rearrange("d j s m -> s d j m")), [B["selrep"]], [b_selr])
                gb, b_gb = self.TB(c, "s_gb", [128, 16], F32)
                S.dma(S.sp, lambda e: e.dma_start(out=gb[:], in_=dr["ml_gate_b"][layer:layer + 1, :].partition_broadcast(128)), [B["ml_gate_b"]], [b_gb])
            NB = 3
            inq = [self.TB(c, f"s_inq{i}", [128, 512], BF16) for i in range(NB)]
            inv = [self.TB(c, f"s_inv{i}", [128, 512], BF16) for i in range(NB)]
            if kind == "hg":
                inz = [self.TB(c, f"s_inz{i}", [128, 512], F32) for i in range(NB)]
            else:
                ink = [self.TB(c, f"s_ink{i}", [128, 512], BF16) for i in range(NB)]
                ing = [self.TB(c, f"s_ing{i}", [128, 16], F32) for i in range(NB)]
                inr = [self.TB(c, f"s_inr{i}", [128, 4, 32], F32) for i in range(NB)]
            ing2 = [self.TB(c, f"s_ing2{i}", [128, 512], BF16) for i in range(NB)]
            inof = [self.TB(c, f"s_inof{i}", [128, 512], F32) for i in range(NB)]
            f1, b_f1 = self.TB(c, "s_f1", [128, 512], F32)
            f2, b_f2 = self.TB(c, "s_f2", [128, 512], F32)
            f3, b_f3 = self.TB(c, "s_f3", [128, 512], F32)
            f4, b_f4 = self.TB(c, "s_f4", [128, 512], F32)
            f5, b_f5 = self.TB(c, "s_f5", [128, 512], F32)
            lf, b_lf = self.TB(c, "s_lf", [128, 512], F32)
            sm, b_sm = self.TB(c, "s_sm", [128, 64], F32)
            sc_, b_sc = self.TB(c, "s_sc", [128, 64], F32)
            qt = [self.TB(c, f"s_qt{i}", [128, 512], BF16) for i in range(NB)]
            kk = [self.TB(c, f"s_kk{i}", [128, 512], BF16) for i in range(NB)]
            vv = [self.TB(c, f"s_vv{i}", [128, H, DV], BF16) for i in range(NB)]
            qkT = [self.TB(c, f"s_qkT{i}", [128, 8, 128], BF16) for i in range(NB)]
            fac = [self.TB(c, f"s_fac{i}", [128, H, 6], F32) for i in range(NB)]
            Am, b_Am = self.TB(c, "s_Am", [128, H * 128], BF16)
            St, b_St = self.TB(c, "s_St", [128, H, DV], F32)
            Sp, b_Sp = self.TB(c, "s_Sp", [128, H, DV], BF16)
            kvt, b_kvt = self.TB(c, "s_kvt", [128, H, DV], F32)
            osbs = [self.TB(c, f"s_osb{i}", [128, 512], F32) for i in range(2)]
            sr, b_sr = self.TB(c, "s_sr", [128, 64], F32)
            ro1, b_ro1 = self.TB(c, "s_ro1", [128, 512], F32)
            ro2, b_ro2 = self.TB(c, "s_ro2", [128, 512], F32)
            rob, b_rob = self.TB(c, "s_rob", [128, 512], BF16)
            brs, b_brs = self.TB(c, "s_brs", [128, 4, 128], BF16)
            if kind == "ml":
                for i in range(NB):
                    S.op(S.dve, lambda e: e.memset(vv[i][0][:, :, 128:129], 1.0), [], [vv[i][1]])
            pX, bX = ps[0], psb[0]
            pT, bT = ps[1][:].bitcast(BF16), psb[1]
            pA, bA = ps[2], psb[2]
            pO = [ps[3], ps[4]]; bO = [psb[3], psb[4]]
            pK = [ps[5], ps[6]]; bK = [psb[5], psb[6]]
            pR, bR = ps[7], psb[7]
            pF, bF = ps[7], Buf("psF")
            pOv = [p[:, 0:2 * DV].rearrange("p (h d) -> p h d", h=2) for p in pO]
            pKv = [p[:, 0:2 * DV].rearrange("p (h d) -> p h d", h=2) for p in pK]
            names = {"hg": ("p_hgq", ("p_hgf", "p_hgb"), "p_hgi", "p_hgg"), "ml": ("p_mlq", "p_mlk", "p_mlv", "p_mlo")}[kind]

            def exp_(out, in_, rd, wr, scale=1.0, bias=None):
                if bias is None:
                    S.op(S.act, lambda e: e.activation(out=out, in_=in_, func=AF.Exp, scale=scale), rd, wr)
                    yield
                else:
                    S.op(S.act, lambda e: e.activation(out=out, in_=in_, func=AF.Exp, scale=scale, bias=bias), rd, wr)
                    yield

            def load(slot, ti, d):
                rows = slice(ti * 128, (ti + 1) * 128)
                S.dma(S.sp, lambda e: e.dma_start(out=inq[slot][0][:], in_=dr[names[0]][rows, :]), [B[names[0]]], [inq[slot][1]])
                S.dma(S.sp, lambda e: e.dma_start(out=inv[slot][0][:], in_=dr[names[2]][rows, :]), [B[names[2]]], [inv[slot][1]])
                if kind == "hg":
                    zn = names[1][d]
                    S.dma(S.sp, lambda e: e.dma_start(out=inz[slot][0][:], in_=dr[zn][rows, :]), [B[zn]], [inz[slot][1]])
                else:
                    S.dma(S.sp, lambda e: e.dma_start(out=ink[slot][0][:], in_=dr[names[1]][rows, :]), [B[names[1]]], [ink[slot][1]])
                    S.dma(S.sp, lambda e: e.dma_start(out=ing[slot][0][:], in_=dr["p_mlg"][rows, :]), [B["p_mlg"]], [ing[slot][1]])
                    if ti >= 2:
                        S.dma(S.sp, lambda e: e.dma_start(out=inr[slot][0][:], in_=dr["rope"][(ti - 2) * 128:(ti - 1) * 128, :, :]),
                              [B["rope"]], [inr[slot][1]])
                if d == 1 and not (last and ti < 2):
                    S.dma(S.sp, lambda e: e.dma_start(out=ing2[slot][0][:], in_=dr[names[3]][rows, :]), [B[names[3]]], [ing2[slot][1]])
                    S.dma(S.sp, lambda e: e.dma_start(out=inof[slot][0][:], in_=dr[of_name][rows, :]), [B[of_name]], [inof[slot][1]])

            def prep_hg(slot, ti, d):
                q, bq = inq[slot]; z, bz = inz[slot]; v, bv = inv[slot]
                yield from exp_(f1[:], z[:], [bz], [b_f1], scale=-1.0)
                S.op(S.act, lambda e: e.activation(out=f1[:], in_=f1[:], func=AF.Ln, bias=1.0), [b_f1], [b_f1])
                yield
                yield from exp_(f2[:], f1[:], [b_f1], [b_f2], scale=-1.0)
                S.op(S.dve, lambda e: e.tensor_tensor(out=f2[:], in0=f2[:], in1=oml[:, d, :], op=ALU.mult), [b_f2, b_oml], [b_f2])
                yield
                S.op(S.dve, lambda e: e.scalar_tensor_tensor(out=f3[:], in0=f2[:], scalar=1e-30, in1=lb[:, d, :], op0=ALU.max, op1=ALU.add),
                     [b_f2, b_lb], [b_f3])
                yield
                S.op(S.act, lambda e: e.activation(out=lf[:], in_=f3[:], func=AF.Ln), [b_f3], [b_lf])
                yield
                S.op(S.pool, lambda e: e.tensor_tensor(out=f3[:], in0=oml[:, d, :], in1=f2[:], op=ALU.subtract), [b_oml, b_f2, b_lf], [b_f3])
                yield
                S.op(S.pe, lambda e: e.matmul(pX[:, :], lhsT=mx[:, d, :], rhs=lf[:], start=True, stop=True), [b_mx, b_lf], [bX])
                yield
                for h in range(H):
                    S.op(S.pe, lambda e: e.matmul(pF[:, 256 + h * 6:256 + h * 6 + 6], lhsT=lf[:, h * 128:(h + 1) * 128], rhs=selt[:, d, :],
                                                  start=True, stop=True), [b_lf, b_sel], [bF], inc=(h == H - 1))
                    yield
                fc, bfc = fac[slot]
                yield from exp_(fc[:].rearrange("p h j -> p (h j)"), pF[:, 256:256 + H * 6], [bF], [bfc])
                yield from exp_(f1[:], q[:], [bq], [b_f1], scale=-1.0)
                S.op(S.act, lambda e: e.activation(out=f1[:], in_=f1[:], func=AF.Ln, bias=1.0), [b_f1], [b_f1])
                yield
                yield from exp_(f2[:], f1[:], [b_f1], [b_f2], scale=-1.0)
                S.op(S.pool, lambda e: e.tensor_tensor(out=f2[:], in0=f2[:], in1=q[:], op=ALU.mult), [b_f2, bq], [b_f2])
                yield
                yield from exp_(f4[:], pX[:, :], [bX], [b_f4])
                yield from exp_(f5[:], pX[:, :], [bX], [b_f5], scale=-1.0)
                S.op(S.dve, lambda e: e.tensor_tensor(out=qt[slot][0][:], in0=f2[:], in1=f4[:], op=ALU.mult), [b_f2, b_f4], [qt[slot][1]])
                yield
                S.op(S.dve, lambda e: e.tensor_tensor(out=kk[slot][0][:], in0=f3[:], in1=f5[:], op=ALU.mult), [b_f3, b_f5], [kk[slot][1]])
                yield
                S.op(S.pool, lambda e: e.tensor_copy(out=vv[slot][0][:].rearrange("p h d -> p (h d)"), in_=v[:]), [bv], [vv[slot][1]])
                yield

            def rope_(dst, src, tab, rd, wr):
                sv = src[:].rearrange("p (h a b f) -> p h a b f", h=H, a=2, b=2)
                dv_ = dst[:].rearrange("p (h a b f) -> p h a b f", h=H, a=2, b=2)
                t1v = f1[:].rearrange("p (h a b f) -> p h a b f", h=H, a=2, b=2)
                tb = tab[:].rearrange("p (a b) f -> p a b f", a=2)
                for a in range(2):
                    cosb = tb[:, a, 0, :].unsqueeze(1).to_broadcast([128, H, 32])
                    sinb = tb[:, a, 1, :].unsqueeze(1).to_broadcast([128, H, 32])
                    p1, p2 = sv[:, :, a, 0, :], sv[:, :, a, 1, :]
                    S.op(S.dve, lambda e: e.tensor_tensor(out=t1v[:, :, a, 0, :], in0=p1, in1=cosb, op=ALU.mult), rd, [b_f1])
                    yield
                    S.op(S.pool, lambda e: e.tensor_tensor(out=t1v[:, :, a, 1, :], in0=p2, in1=sinb, op=ALU.mult), rd, [b_f1])
                    yield
                    S.op(S.dve, lambda e: e.tensor_tensor(out=dv_[:, :, a, 0, :], in0=t1v[:, :, a, 0, :], in1=t1v[:, :, a, 1, :], op=ALU.subtract), [b_f1], wr)
                    yield
                    S.op(S.pool, lambda e: e.tensor_tensor(out=t1v[:, :, a, 0, :], in0=p1, in1=sinb, op=ALU.mult), rd + wr, [b_f1])
                    yield
                    S.op(S.dve, lambda e: e.tensor_tensor(out=t1v[:, :, a, 1, :], in0=p2, in1=cosb, op=ALU.mult), rd, [b_f1])
                    yield
                    S.op(S.pool, lambda e: e.tensor_tensor(out=dv_[:, :, a, 1, :], in0=t1v[:, :, a, 0, :], in1=t1v[:, :, a, 1, :], op=ALU.add), [b_f1], wr)
                    yield

            def prep_ml(slot, ti, d):
                q, bq = inq[slot]; k, bk = ink[slot]; v, bv = inv[slot]; g, bg = ing[slot]
                S.op(S.dve, lambda e: e.tensor_tensor(out=sm[:, 0:16], in0=g[:], in1=gb[:], op=ALU.add), [bg, b_gb], [b_sm])
                yield
                gi = sm[:, d * 8:d * 8 + 4]
                gf = sm[:, d * 8 + 4:d * 8 + 8]
                yield from exp_(sm[:, 16:20], gf, [b_sm], [b_sm], scale=-1.0)
                S.op(S.act, lambda e: e.activation(out=sm[:, 20:24], in_=sm[:, 16:20], func=AF.Ln, bias=1.0), [b_sm], [b_sm])
                yield
                S.op(S.dve, lambda e: e.tensor_scalar(out=lf[:, 0:4], in0=sm[:, 20:24], scalar1=-1.0, scalar2=None, op0=ALU.mult), [b_sm], [b_lf])
                yield
                S.op(S.pe, lambda e: e.matmul(pX[:, 0:4], lhsT=mx[:, d, :], rhs=lf[:, 0:4], start=True, stop=True), [b_mx, b_lf], [bX])
                yield
                for j in range(6):
                    S.op(S.pe, lambda e: e.matmul(pF[:, 256 + j * 4:256 + j * 4 + 4], lhsT=selr[:, d, j, :], rhs=lf[:, 0:4], start=True, stop=True),
                         [b_selr, b_lf], [bF], inc=(j == 5))
                    yield
                fc, bfc = fac[slot]
                yield from exp_(fc[:].rearrange("p h j -> p j h"), pF[:, 256:280].rearrange("p (j h) -> p j h", j=6), [bF], [bfc])
                yield from exp_(sm[:, 24:28], pX[:, 0:4], [bX], [b_sm])
                S.op(S.dve, lambda e: e.tensor_tensor(out=sm[:, 28:32], in0=gi, in1=pX[:, 0:4], op=ALU.subtract), [b_sm, bX], [b_sm])
                yield
                yield from exp_(sm[:, 32:36], sm[:, 28:32], [b_sm], [b_sm], bias=None)
                S.op(S.dve, lambda e: e.tensor_scalar(out=sm[:, 32:36], in0=sm[:, 32:36], scalar1=float(128 ** -0.5), scalar2=None, op0=ALU.mult), [b_sm], [b_sm])
                yield
                if ti >= 2:
                    yield from rope_(f2, q, inr[slot][0], [bq, inr[slot][1]], [b_f2])
                    yield from rope_(f3, k, inr[slot][0], [bk, inr[slot][1]], [b_f3])
                    qs, bqs, ks, bks = f2, b_f2, f3, b_f3
                else:
                    qs, bqs, ks, bks = q, bq, k, bk
                eq = sm[:, 24:28].unsqueeze(2).to_broadcast([128, H, 128])
                ek = sm[:, 32:36].unsqueeze(2).to_broadcast([128, H, 128])
                S.op(S.dve, lambda e: e.tensor_tensor(out=qt[slot][0][:].rearrange("p (h d) -> p h d", h=H),
                                                      in0=qs[:].rearrange("p (h d) -> p h d", h=H), in1=eq, op=ALU.mult), [bqs, b_sm], [qt[slot][1]])
                yield
                S.op(S.dve, lambda e: e.tensor_tensor(out=kk[slot][0][:].rearrange("p (h d) -> p h d", h=H),
                                                      in0=ks[:].rearrange("p (h d) -> p h d", h=H), in1=ek, op=ALU.mult), [bks, b_sm], [kk[slot][1]])
                yield
                S.op(S.pool, lambda e: e.tensor_copy(out=vv[slot][0][:, :, 0:128], in_=v[:].rearrange("p (h d) -> p h d", h=H)), [bv], [vv[slot][1]])
                yield

            def core(slot, ti, d, osb, b_osb):
                q_, bq_ = qt[slot]; k_, bk_ = kk[slot]; v_, bv_ = vv[slot]; T_, bT_ = qkT[slot]; fc, bfc = fac[slot]
                for h in range(H):
                    S.op(S.pe, lambda e: e.transpose(pT[:, h * 128:(h + 1) * 128], q_[:, h * 128:(h + 1) * 128], idb[:]), [bq_, b_id], [bT], inc=False)
                    yield
                for h in range(H):
                    S.op(S.pe, lambda e: e.transpose(pT[:, (4 + h) * 128:(5 + h) * 128], k_[:, h * 128:(h + 1) * 128], idb[:]), [bk_, b_id], [bT], inc=(h == H - 1))
                    yield
                S.op(S.act, lambda e: e.copy(out=T_[:].rearrange("p j t -> p (j t)"), in_=pT[:, :]), [bT], [bT_])
                yield
                for h in range(H):
                    S.op(S.pe, lambda e: e.matmul(pA[:, h * 128:(h + 1) * 128], lhsT=T_[:, 4 + h, :], rhs=T_[:, h, :], start=True, stop=True),
                         [bT_], [bA], inc=(h == H - 1))
                    yield
                S.op(S.pool, lambda e: e.memset(Am[:], 0.0), [], [b_Am])
                yield
                S.op(S.dve, lambda e: e.copy_predicated(out=Am[:], mask=am[:, d, :].bitcast(U32), data=pA[:, :]), [bA, b_am], [b_Am])
                yield
                for cc in ((0, 1) if d == 0 else (1, 0)):
                    rs = slice(cc * 64, (cc + 1) * 64)
                    S.op(S.dve, lambda e: e.tensor_tensor(out=Sp[:], in0=St[:], in1=fc[:, :, cc * 3:cc * 3 + 1].to_broadcast([128, H, DV]), op=ALU.mult),
                         [b_St, bfc], [b_Sp])
                    yield
                    for h in range(H):
                        S.op(S.pe, lambda e: e.matmul(pOv[h // 2][rs, h % 2, :], lhsT=Am[rs, h * 128 + cc * 64:h * 128 + (cc + 1) * 64], rhs=v_[rs, h, :], start=True, stop=False),
                             [b_Am, bv_], [bO[h // 2]], inc=False)
                        yield
                        S.op(S.pe, lambda e: e.matmul(pOv[h // 2][rs, h % 2, :], lhsT=T_[:, h, rs], rhs=Sp[:, h, :], start=False, stop=True),
                             [bT_, b_Sp], [bO[h // 2]], inc=(h % 2 == 1))
                        yield
                    for h in range(H):
                        S.op(S.pe, lambda e: e.matmul(pKv[h // 2][:, h % 2, :], lhsT=k_[rs, h * 128:(h + 1) * 128], rhs=v_[rs, h, :], start=True, stop=True),
                             [bk_, bv_], [bK[h // 2]], inc=(h % 2 == 1))
                        yield
                    for j in range(2):
                        S.op(S.dve, lambda e: e.tensor_tensor(out=kvt[:, 2 * j:2 * j + 2, :], in0=pKv[j][:, :, :],
                                                              in1=fc[:, 2 * j:2 * j + 2, cc * 3 + 2:cc * 3 + 3].to_broadcast([128, 2, DV]), op=ALU.mult),
                             [bK[j], bfc], [b_kvt])
                        yield
                    S.op(S.pool, lambda e: e.tensor_tensor(out=St[:], in0=St[:], in1=fc[:, :, cc * 3 + 1:cc * 3 + 2].to_broadcast([128, H, DV]), op=ALU.mult),
                         [b_St, bfc], [b_St])
                    yield
                    S.op(S.dve, lambda e: e.tensor_tensor(out=St[:], in0=St[:], in1=kvt[:], op=ALU.add), [b_St, b_kvt], [b_St])
                    yield
                if kind == "hg":
                    for j in range(2):
                        S.op(S.act, lambda e: e.copy(out=osb[:, j * 256:(j + 1) * 256], in_=pO[j][:, 0:256]), [bO[j]], [b_osb])
                        yield
                else:
                    for j in range(2):
                        den = pOv[j][:, :, 128:129]
                        S.op(S.dve, lambda e: e.tensor_scalar(out=sc_[:, 36 + 2 * j:38 + 2 * j].unsqueeze(2), in0=den, scalar1=-1.0, scalar2=None, op0=ALU.mult),
                             [bO[j]], [b_sc])
                        yield
                        S.op(S.dve, lambda e: e.scalar_tensor_tensor(out=sc_[:, 40 + 2 * j:42 + 2 * j].unsqueeze(2), in0=den, scalar=1.0,
                                                                     in1=sc_[:, 36 + 2 * j:38 + 2 * j].unsqueeze(2), op0=ALU.max, op1=ALU.max),
                             [bO[j], b_sc], [b_sc])
                        yield
                    S.op(S.dve, lambda e: e.reciprocal(out=sc_[:, 44:48], in_=sc_[:, 40:44]), [b_sc], [b_sc])
                    yield
                    for j in range(2):
                        S.op(S.dve, lambda e: e.tensor_tensor(out=osb[:, j * 256:(j + 1) * 256].rearrange("p (h d) -> p h d", h=2), in0=pOv[j][:, :, 0:128],
                                                              in1=sc_[:, 44 + 2 * j:46 + 2 * j].unsqueeze(2).to_broadcast([128, 2, 128]), op=ALU.mult), [bO[j], b_sc], [b_osb])
                        yield

            def readout(slot, ti, osb, b_osb):
                g2, bg2 = ing2[slot]; of_, bof = inof[slot]
                S.op(S.dve, lambda e: e.tensor_tensor(out=ro1[:], in0=osb[:], in1=of_[:], op=ALU.add), [b_osb, bof], [b_ro1])
                yield
                for h in range(H):
                    S.op(S.act, lambda e: e.activation(out=ro2[:, h * 128:(h + 1) * 128], in_=ro1[:, h * 128:(h + 1) * 128], func=AF.Square,
                                                       accum_out=sr[:, 48 + h:49 + h]), [b_ro1], [b_ro2, b_sr])
                    yield
                S.op(S.dve, lambda e: e.tensor_scalar(out=sr[:, 52:56], in0=sr[:, 48:52], scalar1=1.0 / 128, scalar2=EPS, op0=ALU.mult, op1=ALU.add), [b_sr], [b_sr])
                yield
                S.op(S.act, lambda e: e.activation(out=sr[:, 56:60], in_=sr[:, 52:56], func=AF.Ln), [b_sr], [b_sr])
                yield
                yield from exp_(sr[:, 60:64], sr[:, 56:60], [b_sr], [b_sr], scale=-0.5)
                S.op(S.dve, lambda e: e.tensor_tensor(out=ro2[:].rearrange("p (h d) -> p h d", h=H), in0=ro1[:].rearrange("p (h d) -> p h d", h=H),
                                                      in1=sr[:, 60:64].unsqueeze(2).to_broadcast([128, H, 128]), op=ALU.mult), [b_ro1, b_sr], [b_ro2])
                yield
                S.op(S.pool, lambda e: e.tensor_tensor(out=ro2[:], in0=ro2[:], in1=nw[:], op=ALU.mult), [b_ro2, b_nw], [b_ro2])
                yield
                yield from exp_(ro1[:], g2[:], [bg2], [b_ro1], scale=-1.0)
                S.op(S.act, lambda e: e.activation(out=ro1[:], in_=ro1[:], func=AF.Ln, bias=1.0), [b_ro1], [b_ro1])
                yield
                yield from exp_(ro1[:], ro1[:], [b_ro1], [b_ro1], scale=-1.0)
                if kind == "hg":
                    S.op(S.pool, lambda e: e.tensor_tensor(out=ro1[:], in0=ro1[:], in1=g2[:], op=ALU.mult), [b_ro1, bg2], [b_ro1])
                    yield
                S.op(S.dve, lambda e: e.tensor_tensor(out=rob[:], in0=ro2[:], in1=ro1[:], op=ALU.mult), [b_ro1, b_ro2], [b_rob])
                yield
                pRb = pR[:].bitcast(BF16)
                for j in range(4):
                    S.op(S.pe, lambda e: e.transpose(pRb[:, j * 128:(j + 1) * 128], rob[:, j * 128:(j + 1) * 128], idb[:]), [b_rob, b_id], [bR], inc=(j == 3))
                    yield
                S.op(S.act, lambda e: e.copy(out=brs[:].rearrange("p j t -> p (j t)"), in_=pRb[:, 0:512]), [bR], [b_brs])
                yield
                S.dma(S.sp, lambda e: e.dma_start(out=dr["brT"][br_idx, :, ti * 128:(ti + 1) * 128].rearrange("(j p) t -> p j t", p=128), in_=brs[:]),
                      [b_brs], [B["brT"]])
                yield

            prep = prep_hg if kind == "hg" else prep_ml

            def chain(*gens):
                for g in gens:
                    yield from g

            def store_fwd(ti, osb, b_osb):
                S.dma(S.sp, lambda e: e.dma_start(out=dr[of_name][ti * 128:(ti + 1) * 128, :], in_=osb[:]), [b_osb], [B[of_name]])
                yield

            def interleave(*gens):
                gens = [iter(g) for g in gens]
                done = [False] * len(gens)
                while not all(done):
                    for k, g in enumerate(gens):
                        if not done[k]:
                            try:
                                next(g)
                            except StopIteration:
                                done[k] = True

            for d in range(2):
                order = list(range(NT)) if d == 0 else [1, 0] + list(range(NT - 1, 1, -1))
                S.op(S.dve, lambda e: e.memset(St[:], 0.0), [], [b_St])
                load(0, order[0], d)
                if len(order) > 1:
                    load(1, order[1], d)
                for _ in prep(0, order[0], d):
                    pass
                pend = iter(())
                for i, ti in enumerate(order):
                    slot = i % NB
                    osb, b_osb = osbs[i % 2]
                    work = [core(slot, ti, d, osb, b_osb)]
                    if d == 0:
                        work.append(store_fwd(ti, osb, b_osb))
                    nxt = prep((i + 1) % NB, order[i + 1], d) if i + 1 < len(order) else iter(())
                    interleave(nxt, chain(*work), pend)
                    if i + 2 < len(order):
                        load((i + 2) % NB, order[i + 2], d)
                    if d == 1 and not (last and ti < 2):
                        pend = readout(slot, ti, osb, b_osb)
                    else:
                        pend = iter(())
                for _ in pend:
                    pass
            S.barrier()

    def declare_na(self):
        self.din("nabias", [DEPTH, 5, 8, 128, 5, 128])

    def stage_na(self, layer, last):
        nc, S, B, ps, psb, dr = self.nc, self.S, self.bufs, self.ps, self.psb, self.dram
        with ExitStack() as c:
            idb, b_id = self.TB(c, "a_id", [128, 128], BF16)
            S.dma(S.pool, lambda e: e.dma_start(out=idb[:], in_=dr["ident"][:, :]), [B["ident"]], [b_id])
            kT, _ = self.TB(c, "a_kT", [128, 4, T], BF16)
            KCH = 8
            b_kT = [Buf(f"kT{i}") for i in range((NT + KCH - 1) // KCH)]
            vN, _ = self.TB(c, "a_vN", [128, NT, 8, 65], BF16)
            b_vN = [Buf(f"vN{i}") for i in range(NT)]
            S.op(S.dve, lambda e: e.memset(vN[:, :, :, 64:65], 1.0), [], b_vN)

            def load_k(ci):
                a_, b_ = ci * KCH * 128, min(NT, (ci + 1) * KCH) * 128
                S.dma(S.sp, lambda e: e.dma_start(out=kT[:, :, a_:b_], in_=dr["p_naqk"][512:1024, a_:b_].rearrange("(j p) t -> p j t", p=128)),
                      [B["p_naqk"]], [b_kT[ci]])

            def load_v(t_):
                S.dma(S.sp, lambda e: e.dma_start(out=vN[:, t_, :, 0:64],
                                                  in_=dr["p_nav"][t_ * 128:(t_ + 1) * 128, :].rearrange("p (h d) -> p h d", h=8)),
                      [B["p_nav"]], [b_vN[t_]])

            for ci in range(len(b_kT)):
                load_k(ci)
                for t_ in range(ci * KCH, min(NT, (ci + 1) * KCH)):
                    load_v(t_)
            bias_i, b_bi = self.TB(c, "a_bi", [128, 8, 640], F32)
            S.dma(S.sp, lambda e: e.dma_start(out=bias_i[:].rearrange("p h (j q) -> p h j q", j=5),
                                                in_=dr["nabias"][layer, 0].rearrange("h p j q -> p h j q")), [B["nabias"]], [b_bi])
            bias_e, b_be = self.TB(c, "a_be", [128, 8, 640], F32)
            qq = [self.TB(c, f"a_qq{i}", [128, 4, 128], BF16) for i in range(2)]
            sc = [self.TB(c, f"a_sc{i}", [128, 640], F32) for i in range(3)]
            pt = [self.TB(c, f"a_pt{i}", [128, 896], BF16) for i in range(3)]
            rc, b_rc = self.TB(c, "a_rc", [128, 8], F32)
            no, b_no = self.TB(c, "a_no", [128, 512], BF16)
            brs, b_brs = self.TB(c, "a_brs", [128, 4, 128], BF16)
            pS = [self.psall[:, 0:1024], self.psall[:, 1024:2048], self.psall[:, 2048:3072]]
            bS = [Buf("naS0"), Buf("naS1"), Buf("naS2")]
            pO = [ps[7][:, 0:128], ps[7][:, 128:256], ps[7][:, 256:384]]
            bO = [Buf("naO0"), Buf("naO1"), Buf("naO2")]
            pR, bR = ps[6][:].bitcast(BF16), psb[6]
            qbs = ([] if last else [("ctx", 0), ("ctx", 1)]) + [("lat", i) for i in range(32)]
            it = 0

            def loadq(i):
                kind, qb = qbs[i]
                ti = qb if kind == "ctx" else 2 + qb
                S.dma(S.sp, lambda e: e.dma_start(out=qq[i % 2][0][:], in_=dr["p_naqk"][0:512, ti * 128:(ti + 1) * 128].rearrange("(j p) t -> p j t", p=128)),
                      [B["p_naqk"]], [qq[i % 2][1]])

            loadq(0)
            for i, (kind, qb) in enumerate(qbs):
                if i + 1 < len(qbs):
                    loadq(i + 1)
                q_, bq_ = qq[i % 2]
                ti = qb if kind == "ctx" else 2 + qb
                if kind == "lat":
                    R0 = min(max(2 * qb - 4, 0), 54)
                    kt0 = 2 + R0 // 2
                    cls = {0: 1, 1: 2, 30: 3, 31: 4}.get(qb, 0)
                    if cls:
                        S.dma(S.sp, lambda e: e.dma_start(out=bias_e[:].rearrange("p h (j q) -> p h j q", j=5),
                                                            in_=dr["nabias"][layer, cls].rearrange("h p j q -> p h j q")), [B["nabias"]], [b_be])
                        bt, bbt = bias_e, b_be
                    else:
                        bt, bbt = bias_i, b_bi
                    kts = [kt0 + j for j in range(5)] + [0, 1]
                else:
                    kts = [0, 1]
                nk = len(kts)
                for hp in range(4):
                    slots = []
                    for hh in range(2):
                        slots.append((pS[it % 3], bS[it % 3], sc[it % 3], pt[it % 3], pO[it % 3], bO[it % 3]))
                        it += 1
                    for j, kt in enumerate(kts):
                        for hh in range(2):
                            base = hh * 64
                            s_, bs_ = slots[hh][0], slots[hh][1]
                            S.op(S.pe, lambda e: e.matmul(s_[:, j * 128:(j + 1) * 128], lhsT=kT[base:base + 64, hp, kt * 128:(kt + 1) * 128],
                                                          rhs=q_[base:base + 64, hp, :], start=True, stop=True), [b_kT[kt // KCH], bq_], [bs_], inc=(j == nk - 1))
                    for hh in range(2):
                        h = 2 * hp + hh
                        s_, bs_, (sc_, bsc_), (p_, bp_), o_, bo_ = slots[hh]
                        if kind == "lat":
                            S.op(S.dve, lambda e: e.scalar_tensor_tensor(out=sc_[:], in0=s_[:, 0:640], scalar=0.125, in1=bt[:, h, :], op0=ALU.mult, op1=ALU.add),
                                 [bs_, bbt], [bsc_])
                            S.op(S.act, lambda e: e.activation(out=p_[:, 0:640], in_=sc_[:], func=AF.Exp), [bsc_], [bp_])
                            S.op(S.act, lambda e: e.activation(out=p_[:, 640:896], in_=s_[:, 640:896], func=AF.Exp, scale=0.125), [bs_], [bp_])
                        else:
                            S.op(S.act, lambda e: e.activation(out=p_[:, 0:256], in_=s_[:, 0:256], func=AF.Exp, scale=0.125), [bs_], [bp_])
                        for j, kt in enumerate(kts):
                            S.op(S.pe, lambda e: e.matmul(o_[:, 0:65], lhsT=p_[:, j * 128:(j + 1) * 128], rhs=vN[:, kt, h, :], start=(j == 0), stop=(j == nk - 1)),
                                 [bp_, b_vN[kt]], [bo_], inc=(j == nk - 1))
                        S.op(S.dve, lambda e: e.reciprocal(out=rc[:, h:h + 1], in_=o_[:, 64:65]), [bo_], [b_rc])
                        S.op(S.act, lambda e: e.activation(out=no[:, h * 64:(h + 1) * 64], in_=o_[:, 0:64], func=AF.Copy, scale=rc[:, h:h + 1]), [bo_, b_rc], [b_no])
                for j in range(4):
                    S.op(S.pe, lambda e: e.transpose(pR[:, j * 128:(j + 1) * 128], no[:, j * 128:(j + 1) * 128], idb[:]), [b_no, b_id], [bR], inc=(j == 3))
                S.op(S.dve, lambda e: e.tensor_copy(out=brs[:].rearrange("p j t -> p (j t)"), in_=pR[:, 0:512]), [bR], [b_brs])
                S.dma(S.sp, lambda e: e.dma_start(out=dr["brT"][0, :, ti * 128:(ti + 1) * 128].rearrange("(j p) t -> p j t", p=128), in_=brs[:]),
                      [b_brs], [B["brT"]])
            S.barrier()


    def declare_tail(self):
        self.din("w_branch", [DEPTH, 3, 512, D])
        self.din("w_out", [DEPTH, D, D])
        self.din("ffn_w_up", [1, D, 2 * FFN])
        self.din("ffn_w_down", [1, FFN, D])
        self.din("moe_router_t", [1, NEXP, D])
        self.din("moe_w_up", [1, NEXP, D, 2 * EDIM])
        self.din("moe_w_down", [1, NEXP, EDIM, D])
        self.din("final_norm_w", [D])
        self.dscr("glu_acc", [T, D])
        self.acc_bufs = [Buf(f"acc{i}") for i in range(NT)]
        self.h_bufs = [Buf(f"hb{i}") for i in range(NT)]
        self.dout("out", [SEQ // 2, D])
        self.din("tokidx", [128, 16], U32)

    def stage_merge(self, layer, aT, aT_bufs, last):
        nc, S, B, ps, psb, dr = self.nc, self.S, self.bufs, self.ps, self.psb, self.dram
        t0 = 2 if last else 0
        hsrc = "xin" if layer == 0 else "hbuf"
        with ExitStack() as c:
            wg, _ = self.TB(c, "g_wg", [128, KC, 3072], BF16)
            b_wg = [Buf(f"wg{j}") for j in range(6)]
            wb, _ = self.TB(c, "g_wb", [128, 12, D], BF16)
            b_wb = [Buf(f"wb{i}") for i in range(3)]
            wo, _ = self.TB(c, "g_wo", [128, KC, D], BF16)
            b_wo = [Buf("wo0"), Buf("wo1")]

            def ld_wg(j):
                S.dma(S.pool, lambda e: e.dma_start(out=wg[:, :, j * 512:(j + 1) * 512],
                                                    in_=dr["w_in"][layer, :, C_BG + j * 512:C_BG + (j + 1) * 512].rearrange("(kc p) n -> p kc n", p=128)),
                      [B["w_in"]], [b_wg[j]])

            def ld_wb(i):
                S.dma(S.pool, lambda e: e.dma_start(out=wb[:, i * 4:(i + 1) * 4, :], in_=dr["w_branch"][layer, i].rearrange("(kc p) n -> p kc n", p=128)),
                      [B["w_branch"]], [b_wb[i]])

            def ld_wo(j):
                S.dma(S.pool, lambda e: e.dma_start(out=wo[:, :, j * 512:(j + 1) * 512],
                                                    in_=dr["w_out"][layer, :, j * 512:(j + 1) * 512].rearrange("(kc p) n -> p kc n", p=128)),
                      [B["w_out"]], [b_wo[j]])

            for i in range(3):
                ld_wg(2 * i)
                ld_wb(i)
            for i in range(3):
                ld_wg(2 * i + 1)
            ld_wo(0)
            ld_wo(1)
            g1 = [self.TB(c, f"g_g1{r}", [128, D], F32) for r in range(2)]
            for r in range(2):
                self.load_row_bcast(g1[r][0][:], layer, r, 2, g1[r][1])
            br = [self.TB(c, "g_br0", [128, 12, 512], BF16)] * 2
            sg = [self.TB(c, f"g_sg{i}", [128, 512], F32) for i in range(2)]
            tm = [self.TB(c, f"g_tm{i}", [128, 512], F32) for i in range(2)]
            ya, b_ya = self.TB(c, "g_ya", [128, 512], F32)
            yT, b_yT = self.TB(c, "g_yT", [128, KC, 512], BF16)
            hin = [self.TB(c, "g_hin0", [128, D], F32)] * 2
            ho = [self.TB(c, "g_ho0", [128, D], F32)] * 2
            blocks = [(tb, min(4, NT - tb)) for tb in range(t0, NT, 4)]

            def loadbr(bi):
                tb, nt_ = blocks[bi]
                for i in range(3):
                    S.dma(S.sp, lambda e: e.dma_start(out=br[bi % 2][0][:, i * 4:(i + 1) * 4, 0:nt_ * 128],
                                                      in_=dr["brT"][i, :, tb * 128:(tb + nt_) * 128].rearrange("(kc p) t -> p kc t", p=128)),
                          [B["brT"]], [br[bi % 2][1]])

            cnt = 0
            hc = 0
            loadbr(0)
            for bi, (tb, nt_) in enumerate(blocks):
                n = nt_ * 128
                tok0 = tb * 128
                br_, bbr_ = br[bi % 2]
                for m in range(8):
                    for i in range(3):
                        pG, bG = ps[cnt % 2], psb[cnt % 2]
                        pZ, bZ = ps[2 + cnt % 2], psb[2 + cnt % 2]
                        sg_, bsg_ = sg[cnt % 2]
                        tm_, btm_ = tm[cnt % 2]
                        cnt += 1
                        for k in range(KC):
                            S.op(S.pe, lambda e: e.matmul(pG[:, 0:n], lhsT=wg[:, k, i * 1024 + m * 128:i * 1024 + (m + 1) * 128], rhs=aT[:, k, tok0:tok0 + n],
                                                          start=(k == 0), stop=(k == KC - 1)), [b_wg[(i * 1024 + m * 128) // 512]] + [aT_bufs[t] for t in range(tb, tb + nt_)], [bG], inc=(k == KC - 1))
                        for k in range(4):
                            S.op(S.pe, lambda e: e.matmul(pZ[:, 0:n], lhsT=wb[:, i * 4 + k, m * 128:(m + 1) * 128], rhs=br_[:, i * 4 + k, 0:n],
                                                          start=(k == 0), stop=(k == 3)), [b_wb[i], bbr_], [bZ], inc=(k == 3))
                        S.op(S.act, lambda e: e.activation(out=sg_[:, 0:n], in_=pG[:, 0:n], func=AF.Sigmoid), [bG], [bsg_])
                        if i == 0:
                            S.op(S.dve, lambda e: e.tensor_tensor(out=ya[:, 0:n], in0=pZ[:, 0:n], in1=sg_[:, 0:n], op=ALU.mult), [bZ, bsg_], [b_ya])
                        else:
                            S.op(S.dve, lambda e: e.tensor_tensor(out=tm_[:, 0:n], in0=pZ[:, 0:n], in1=sg_[:, 0:n], op=ALU.mult), [bZ, bsg_], [btm_])
                            if i == 1:
                                S.op(S.pool, lambda e: e.tensor_tensor(out=ya[:, 0:n], in0=ya[:, 0:n], in1=tm_[:, 0:n], op=ALU.add), [b_ya, btm_], [b_ya])
                            else:
                                S.op(S.pool, lambda e: e.tensor_tensor(out=yT[:, m, 0:n], in0=ya[:, 0:n], in1=tm_[:, 0:n], op=ALU.add), [b_ya, btm_], [b_yT])
                if bi + 1 < len(blocks):
                    loadbr(bi + 1)
                for j in range(nt_):
                    ti = tb + j
                    r = 1 if ti < 2 else 0
                    hi_, bhi_ = hin[hc % 2]
                    ho_, bho_ = ho[hc % 2]
                    hc += 1
                    S.dma(S.sp, lambda e: e.dma_start(out=hi_[:], in_=dr[hsrc][ti * 128:(ti + 1) * 128, :]), [B[hsrc], self.h_bufs[ti]], [bhi_])
                    for half in range(2):
                        pW, bW = ps[4 + (2 * hc + half) % 4], psb[4 + (2 * hc + half) % 4]
                        for k in range(KC):
                            S.op(S.pe, lambda e: e.matmul(pW[:, :], lhsT=yT[:, k, j * 128:(j + 1) * 128], rhs=wo[:, k, half * 512:(half + 1) * 512],
                                                          start=(k == 0), stop=(k == KC - 1)), [b_yT, b_wo[half]], [bW], inc=(k == KC - 1))
                        S.op(S.dve, lambda e: e.tensor_tensor(out=ho_[:, half * 512:(half + 1) * 512], in0=pW[:, :], in1=g1[r][0][:, half * 512:(half + 1) * 512],
                                                              op=ALU.mult), [bW, g1[r][1]], [bho_])
                    S.op(S.pool, lambda e: e.tensor_tensor(out=ho_[:], in0=ho_[:], in1=hi_[:], op=ALU.add), [bho_, bhi_], [bho_])
                    S.dma(S.sp, lambda e: e.dma_start(out=dr["hbuf"][ti * 128:(ti + 1) * 128, :], in_=ho_[:]), [bho_], [self.h_bufs[ti]])
            S.barrier()

    def stage_glu(self, layer, fT, fT_bufs, passes, t0, gates=None, ntile=None, resid=False):
        nc, S, B, ps, psb, dr = self.nc, self.S, self.bufs, self.ps, self.psb, self.dram
        ntile = NT - t0 if ntile is None else ntile
        with ExitStack() as c:
            NCH = max(p["nch"] for p in passes)
            wa = [self.TB(c, f"u_wa{i}", [128, KC, NCH * 128], BF16) for i in range(2)]
            wu = [self.TB(c, f"u_wu{i}", [128, KC, NCH * 128], BF16) for i in range(2)]
            wd = [self.TB(c, f"u_wd{i}", [128, NCH, D], BF16) for i in range(2)]
            sA = [self.TB(c, f"u_sA{i}", [128, 512], BF16) for i in range(2)]
            hm = [self.TB(c, f"u_hm{i}", [128, NCH, 512], BF16) for i in range(2)]
            stg = [self.TB(c, f"u_st{i}", [128, D], F32) for i in range(2)]
            acc = [self.TB(c, f"u_ac{i}", [128, D], F32) for i in range(2)]
            wst = [self.TB(c, f"u_ws{i}", [128, 2688], F32) for i in range(2)]
            if resid:
                g5 = [self.TB(c, f"u_g5{r}", [128, D], F32) for r in range(2)]
                for r in range(2):
                    self.load_row_bcast(g5[r][0][:], layer, r, 5, g5[r][1])
                hres = [self.TB(c, f"u_hr{i}", [128, D], F32) for i in range(2)]
            blocks = [(tb, min(4, t0 + ntile - tb)) for tb in range(t0, t0 + ntile, 4)]
            nblk = len(blocks)
            wcnt = [0]

            def pieces(pi):
                p = passes[pi]
                nch, c0, hid, up, down = p["nch"], p["c0"], p["hid"], p["up"], p["down"]
                i = pi % 2
                out = []
                for (w_, bw_), off in ((wa[i], 0), (wu[i], hid)):
                    for k0 in range(0, KC, 3):
                        kn = min(3, KC - k0)
                        out.append((w_[:, k0:k0 + kn, 0:nch * 128], bw_, up[k0 * 128:(k0 + kn) * 128, off + c0 * 128:off + (c0 + nch) * 128].rearrange("(kc p) n -> p kc n", p=128),
                                    p["upn"], (kn, nch * 128)))
                for j0 in range(0, nch, 2):
                    jn = min(2, nch - j0)
                    out.append((wd[i][0][:, j0:j0 + jn, :], wd[i][1], down[(c0 + j0) * 128:(c0 + j0 + jn) * 128, :].rearrange("(j p) n -> p j n", p=128),
                                p["downn"], (jn, D)))
                return out

            def load_piece(pc):
                dst, bdst, src, srcn, (a, b) = pc
                st_, bst_ = wst[wcnt[0] % 2]
                wcnt[0] += 1
                view = st_[:, 0:a * b].rearrange("p (a b) -> p a b", a=a)
                S.dma(S.sp, lambda e: e.dma_start(out=view, in_=src), [B[srcn]], [bst_])
                S.op(S.pool, lambda e: e.tensor_copy(out=dst, in_=view), [bst_], [bdst])

            for pc in pieces(0):
                load_piece(pc)
            cnt = 0
            tcnt = 0

            def load_acc(ti_, slot):
                S.dma(S.sp, lambda e: e.dma_start(out=acc[slot][0][:], in_=dr["glu_acc"][ti_ * 128:(ti_ + 1) * 128, :]), [self.acc_bufs[ti_]], [acc[slot][1]])
            for pi, p in enumerate(passes):
                nxt = pieces(pi + 1) if pi + 1 < len(passes) else []
                nch = p["nch"]
                (wa_, bwa_), (wu_, bwu_), (wd_, bwd_) = wa[pi % 2], wu[pi % 2], wd[pi % 2]
                for bi, (tb, nt_) in enumerate(blocks):
                    lo, hi = (bi * len(nxt)) // nblk, ((bi + 1) * len(nxt)) // nblk
                    for pc in nxt[lo:hi]:
                        load_piece(pc)
                    n = nt_ * 128
                    tok0 = tb * 128
                    hm_, bhm_ = hm[bi % 2]
                    fb = [fT_bufs[t] for t in range(tb, tb + nt_)]
                    for jc in range(nch):
                        pA, bA = ps[cnt % 2], psb[cnt % 2]
                        pU, bU = ps[2 + cnt % 2], psb[2 + cnt % 2]
                        sA_, bsA_ = sA[cnt % 2]
                        cnt += 1
                        for k in range(KC):
                            S.op(S.pe, lambda e: e.matmul(pA[:, 0:n], lhsT=wa_[:, k, jc * 128:(jc + 1) * 128], rhs=fT[:, k, tok0:tok0 + n],
                                                          start=(k == 0), stop=(k == KC - 1)), [bwa_] + fb, [bA], inc=(k == KC - 1))
                        for k in range(KC):
                            S.op(S.pe, lambda e: e.matmul(pU[:, 0:n], lhsT=wu_[:, k, jc * 128:(jc + 1) * 128], rhs=fT[:, k, tok0:tok0 + n],
                                                          start=(k == 0), stop=(k == KC - 1)), [bwu_] + fb, [bU], inc=(k == KC - 1))
                        S.op(S.act, lambda e: e.activation(out=sA_[:, 0:n], in_=pA[:, 0:n], func=AF.Silu), [bA], [bsA_])
                        S.op(S.dve, lambda e: e.tensor_tensor(out=hm_[:, jc, 0:n], in0=pU[:, 0:n], in1=sA_[:, 0:n], op=ALU.mult), [bU, bsA_], [bhm_])
                    for j in range(nt_):
                        ti = tb + j
                        st_, bst_ = stg[tcnt % 2]
                        ac_, bac_ = acc[tcnt % 2]
                        fin = resid and pi == len(passes) - 1
                        if fin:
                            hr_, bhr_ = hres[tcnt % 2]
                            S.dma(S.sp, lambda e: e.dma_start(out=hr_[:], in_=dr["hbuf"][ti * 128:(ti + 1) * 128, :]), [self.h_bufs[ti]], [bhr_])
                        if pi > 0:
                            if j == 0:
                                load_acc(ti, tcnt % 2)
                            if j + 1 < nt_:
                                load_acc(ti + 1, (tcnt + 1) % 2)
                        for half in range(2):
                            hs = slice(half * 512, (half + 1) * 512)
                            pD, bD = ps[4 + (2 * tcnt + half) % 4], psb[4 + (2 * tcnt + half) % 4]
                            for jc in range(nch):
                                S.op(S.pe, lambda e: e.matmul(pD[:, :], lhsT=hm_[:, jc, j * 128:(j + 1) * 128], rhs=wd_[:, jc, hs],
                                                              start=(jc == 0), stop=(jc == nch - 1)), [bhm_, bwd_], [bD], inc=(jc == nch - 1))
                            if gates is not None:
                                gs = gates[0][:, ti, p["gate"]:p["gate"] + 1]
                                if pi == 0:
                                    S.op(S.act, lambda e: e.activation(out=st_[:, hs], in_=pD[:, :], func=AF.Copy, scale=gs), [bD, gates[1]], [bst_])
                                else:
                                    S.op(S.dve, lambda e: e.scalar_tensor_tensor(out=st_[:, hs], in0=pD[:, :], scalar=gs, in1=ac_[:, hs], op0=ALU.mult, op1=ALU.add),
                                         [bD, gates[1], bac_], [bst_])
                            elif pi == 0:
                                S.op(S.act, lambda e: e.copy(out=st_[:, hs], in_=pD[:, :]), [bD], [bst_])
                            else:
                                S.op(S.dve, lambda e: e.tensor_tensor(out=st_[:, hs], in0=pD[:, :], in1=ac_[:, hs], op=ALU.add), [bD, bac_], [bst_])
                        tcnt += 1
                        if fin:
                            r_ = 1 if ti < 2 else 0
                            S.op(S.dve, lambda e: e.tensor_tensor(out=st_[:], in0=st_[:], in1=g5[r_][0][:], op=ALU.mult), [bst_, g5[r_][1]], [bst_])
                            S.op(S.pool, lambda e: e.tensor_tensor(out=st_[:], in0=st_[:], in1=hr_[:], op=ALU.add), [bst_, bhr_], [bst_])
                            S.dma(S.sp, lambda e: e.dma_start(out=dr["hbuf"][ti * 128:(ti + 1) * 128, :], in_=st_[:]), [bst_], [self.h_bufs[ti]])
                        else:
                            S.dma(S.sp, lambda e: e.dma_start(out=dr["glu_acc"][ti * 128:(ti + 1) * 128, :], in_=st_[:]), [bst_], [self.acc_bufs[ti]])
            S.barrier()

    def stage_final(self, layer, last, gather=None):
        nc, S, B, dr = self.nc, self.S, self.bufs, self.dram
        t0 = 2 if last else 0
        with ExitStack() as c:
            g5 = [self.TB(c, f"f_g5{r}", [128, D], F32) for r in range(2)]
            for r in range(2):
                self.load_row_bcast(g5[r][0][:], layer, r, 5, g5[r][1])
            fw, b_fw = self.TB(c, "f_fw", [128, D], F32)
            S.dma(S.sp, lambda e: e.dma_start(out=fw[:], in_=dr["final_norm_w"].rearrange("(o n) -> o n", o=1).partition_broadcast(128)), [B["final_norm_w"]], [b_fw])
            hi = [self.TB(c, f"f_hi{i}", [128, D], F32) for i in range(2)]
            ac = [self.TB(c, f"f_ac{i}", [128, D], F32) for i in range(2)]
            ot = [self.TB(c, f"f_ot{i}", [128, D], F32) for i in range(2)]
            sq, b_sq = self.TB(c, "f_sq", [128, D], BF16)
            st = [self.TB(c, f"f_st{i}", [128, 4], F32) for i in range(2)]
            tiles = list(range(t0, NT)) if gather is None else list(range(2, 2 + 16))

            def load(i):
                ti = tiles[i]
                if gather is None:
                    S.dma(S.sp, lambda e: e.dma_start(out=hi[i % 2][0][:], in_=dr["hbuf"][ti * 128:(ti + 1) * 128, :]), [self.h_bufs[ti]], [hi[i % 2][1]])
                else:
                    S.dma(S.pool, lambda e: e.indirect_dma_start(out=hi[i % 2][0][:], out_offset=None, in_=dr["hbuf"][:, :],
                                                                 in_offset=bass.IndirectOffsetOnAxis(ap=gather[0][:, ti - 2:ti - 1], axis=0)),
                          [B["hbuf"], gather[1]] + self.h_bufs, [hi[i % 2][1]])
                S.dma(S.sp, lambda e: e.dma_start(out=ac[i % 2][0][:], in_=dr["glu_acc"][ti * 128:(ti + 1) * 128, :]), [self.acc_bufs[ti]], [ac[i % 2][1]])

            load(0)
            for i, ti in enumerate(tiles):
                if i + 1 < len(tiles):
                    load(i + 1)
                r = 1 if ti < 2 else 0
                (h_, bh_), (a_, ba_), (o_, bo_), (s_, bs_) = hi[i % 2], ac[i % 2], ot[i % 2], st[i % 2]
                S.op(S.dve, lambda e: e.tensor_tensor(out=a_[:], in0=a_[:], in1=g5[r][0][:], op=ALU.mult), [ba_, g5[r][1]], [ba_])
                S.op(S.pool, lambda e: e.tensor_tensor(out=o_[:], in0=a_[:], in1=h_[:], op=ALU.add), [ba_, bh_], [bo_])
                if not last:
                    S.dma(S.sp, lambda e: e.dma_start(out=dr["hbuf"][ti * 128:(ti + 1) * 128, :], in_=o_[:]), [bo_], [self.h_bufs[ti]])
                else:
                    S.op(S.act, lambda e: e.activation(out=sq[:], in_=o_[:], func=AF.Square, accum_out=s_[:, 0:1]), [bo_], [b_sq, bs_])
                    S.op(S.dve, lambda e: e.tensor_scalar(out=s_[:, 1:2], in0=s_[:, 0:1], scalar1=1.0 / D, scalar2=EPS, op0=ALU.mult, op1=ALU.add), [bs_], [bs_])
                    S.op(S.act, lambda e: e.activation(out=s_[:, 2:3], in_=s_[:, 1:2], func=AF.Ln), [bs_], [bs_])
                    S.op(S.act, lambda e: e.activation(out=s_[:, 3:4], in_=s_[:, 2:3], func=AF.Exp, scale=-0.5), [bs_], [bs_])
                    S.op(S.dve, lambda e: e.scalar_tensor_tensor(out=o_[:], in0=o_[:], scalar=s_[:, 3:4], in1=fw[:], op0=ALU.mult, op1=ALU.mult), [bo_, bs_, b_fw], [bo_])
                    S.dma(S.sp, lambda e: e.dma_start(out=dr["out"][(ti - 2) * 128:(ti - 1) * 128, :], in_=o_[:]), [bo_], [B["out"]])
            S.barrier()

    def glu_passes(self, layer):
        dr = self.dram
        if layer % 2 == 0:
            i = layer // 2
            return [dict(up=dr["ffn_w_up"][i], down=dr["ffn_w_down"][i], upn="ffn_w_up", downn="ffn_w_down", hid=FFN, c0=c0, nch=n, gate=None)
                    for c0, n in ((0, 6), (6, 6), (12, 5), (17, 5))]
        i = layer // 2
        return [dict(up=dr["moe_w_up"][i, ex], down=dr["moe_w_down"][i, ex], upn="moe_w_up", downn="moe_w_down", hid=EDIM, c0=c0, nch=7, gate=ex)
                for ex in range(NEXP) for c0 in (0, 7, 14, 21)]

    def run_layer(self, layer, aT, aT_bufs, stop_after=None):
        S = self.S
        last = layer == DEPTH - 1
        src = "xin" if layer == 0 else "hbuf"
        if not getattr(self, "mods_done", False):
            self.stage_mod(layer)
        self.stage_norm(layer, 0, src, aT, aT_bufs, list(range(NT)))
        self.stage_inproj(layer, aT, aT_bufs)
        self.stage_na(layer, last)
        self.stage_scan(layer, "hg", last)
        self.stage_scan(layer, "ml", last)
        self.stage_merge(layer, aT, aT_bufs, last)
        if stop_after == "merge":
            return
        t0 = 2 if last else 0
        if layer % 2 == 0:
            self.stage_norm(layer, 1, "hbuf", aT, aT_bufs, list(range(t0, NT)))
            self.stage_glu(layer, aT, aT_bufs, self.glu_passes(layer), t0, resid=(not last))
            if not last:
                return
        else:
            with ExitStack() as c:
                gates, b_gates = self.TB(c, "r_gates", [128, NT, NEXP], F32)
                tix, b_tix = self.TB(c, "r_tix", [128, 16], U32)
                S.dma(S.sp, lambda e: e.dma_start(out=tix[:], in_=self.dram["tokidx"][:, :]), [self.bufs["tokidx"]], [b_tix])
                half_tiles = list(range(2, 2 + 16))
                with ExitStack() as c2:
                    wr, _ = self.TB(c2, "r_wr", [128, NEXP, D], F32)
                    b_wr = [Buf(f"wr{ex}") for ex in range(NEXP)]
                    rs, b_rs = self.TB(c2, "r_rs", [128, 48], F32)
                    junk, b_junk = self.TB(c2, "r_junk", [128, D], F32)
                    for ex in range(NEXP):
                        S.dma(S.sp, lambda e: e.dma_start(out=wr[:, ex, :], in_=self.dram["moe_router_t"][layer // 2, ex:ex + 1, :].partition_broadcast(128)),
                              [self.bufs["moe_router_t"]], [b_wr[ex]])
                    self.stage_norm(layer, 1, "hbuf", aT, aT_bufs, half_tiles, router=(gates, b_gates, wr, b_wr, rs, b_rs, junk, b_junk),
                                    gather=(tix, b_tix))
                self.stage_glu(layer, aT, aT_bufs, self.glu_passes(layer), 2, gates=(gates, b_gates), ntile=16)
                self.stage_final(layer, last, gather=(tix, b_tix))
                return
        self.stage_final(layer, last)

    def build(self):
        nc, S = self.nc, self.S
        self.declare_io()
        c = self.stack
        self.psall = c.enter_context(nc.psum_tensor("psall", [128, 4096], F32))
        self.ps = [self.psall[:, i * 512:(i + 1) * 512] for i in range(8)]
        self.psb = [Buf(f"ps{i}") for i in range(8)]
        aT = self.sb(c, "aT", [128, KC, T], BF16)
        aT_bufs = [Buf(f"aT{i}") for i in range(NT)]
        self.declare_proj()
        self.declare_scan()
        self.declare_na()
        self.declare_tail()
        if self.upto == "l0merge":
            self.run_layer(0, aT, aT_bufs, stop_after="merge")
        elif self.upto == "l0":
            self.run_layer(0, aT, aT_bufs)
        else:
            for layer in range(DEPTH):
                self.stage_mod(layer)
            self.mods_done = True
            for layer in range(DEPTH):
                self.run_layer(layer, aT, aT_bufs)
        if "hbuf" in self.debug:
            o = self.dout("dbg_hbuf", [T, D])
            S.dma(S.sp, lambda e: e.dma_start(out=o[:, :], in_=self.dram["hbuf"][:, :]), self.h_bufs + [self.bufs["hbuf"]], [self.bufs["dbg_hbuf"]])
        return self.finish()

    def finish(self):
        S = self.S
        if "modv" in self.debug:
            o = self.dout("dbg_modv", [DEPTH, 2, 8, D])
            S.dma(S.sp, lambda e: e.dma_start(out=o[:, :, :, :], in_=self.dram["modv"][:, :, :, :]),
                  [self.bufs["modv"]], [self.bufs["dbg_modv"]])
        S.barrier()
        self.stack.close()
        return self.nc


def host_consts():
    m = {}
    sidx = np.arange(128)[:, None]; tidx = np.arange(128)[None, :]
    same = (sidx // 64) == (tidx // 64)
    ref = (tidx // 64) * 64 + 31
    ref2 = (tidx // 64) * 64 + 32
    mxf = same * ((sidx <= tidx).astype(np.float32) - (sidx <= ref).astype(np.float32))
    mxb = same * ((sidx < ref2).astype(np.float32) - (sidx < tidx).astype(np.float32))
    m["mx"] = np.stack([mxf, mxb]).astype(np.float32)
    sel = np.zeros((2, 128, 6), np.float32)
    s1 = np.arange(128)
    for cix in range(2):
        inc = (s1 // 64) == cix
        first = inc & (s1 % 64 <= 31)
        second = inc & (s1 % 64 >= 32)
        sel[0, :, cix * 3 + 0] = first; sel[0, :, cix * 3 + 1] = inc; sel[0, :, cix * 3 + 2] = second
        sel[1, :, cix * 3 + 0] = second; sel[1, :, cix * 3 + 1] = inc; sel[1, :, cix * 3 + 2] = first
    m["sel"] = sel
    m["selrep"] = np.ascontiguousarray(np.broadcast_to(sel.transpose(0, 2, 1)[:, :, :, None], (2, 6, 128, 128))).astype(np.float32)
    am = np.stack([same & ((sidx % 64) <= (tidx % 64)), same & ((sidx % 64) >= (tidx % 64))]).astype(np.float32)
    m["amask"] = np.ascontiguousarray(np.broadcast_to(am[:, :, None, :], (2, 128, 4, 128))).astype(np.float32)
    tt = np.arange(SEQ)
    inv_freq = (10000.0 ** (-np.arange(32, dtype=np.float32) / 32)).astype(np.float32)
    ar = (tt // 64).astype(np.float32)[:, None] * inv_freq
    ac = (tt % 64).astype(np.float32)[:, None] * inv_freq
    m["rope"] = np.stack([np.cos(ar), np.sin(ar), np.cos(ac), np.sin(ac)], axis=1).astype(np.float32)
    return m


_NA_IDX = None


def na_bias_tables(rpb):
    global _NA_IDX
    if _NA_IDX is None:
        idx = np.zeros((5, 128, 5, 128, 2), np.int64)
        msk = np.zeros((5, 128, 5, 128), bool)
        for cls, qb in enumerate((5, 0, 1, 30, 31)):
            R0 = min(max(2 * qb - 4, 0), 54)
            q = np.arange(128); r = 2 * qb + q // 64; qc = q % 64
            r0 = np.clip(r - 4, 0, 56); ws = np.clip(qc - 8, 0, 48)
            for j in range(5):
                k = np.arange(128); kr = R0 + 2 * j + k // 64; kc = k % 64
                inw = ((kr[:, None] >= r0[None, :]) & (kr[:, None] < r0[None, :] + 8) &
                       (kc[:, None] >= ws[None, :]) & (kc[:, None] < ws[None, :] + 16))
                drr = np.clip(kr[:, None] - r[None, :] + 7, 0, 14)
                dcc = np.clip(kc[:, None] - qc[None, :] + 15, 0, 30)
                idx[cls, :, j, :, 0] = drr; idx[cls, :, j, :, 1] = dcc; msk[cls, :, j, :] = inw
        _NA_IDX = (idx, msk)
    idx, msk = _NA_IDX
    g = rpb[:, :, idx[..., 0], idx[..., 1]]
    g = np.where(msk[None, None], g, np.float32(-1e30)).astype(np.float32)
    return np.ascontiguousarray(g.transpose(0, 2, 1, 3, 4, 5))


def host_inputs(inputs, b, g=0):
    x, c, ctx, c_ctx = inputs["x"], inputs["c"], inputs["ctx"], inputs["c_ctx"]
    m = {}
    m["xin"] = np.ascontiguousarray(np.concatenate([ctx[b], x[b]], axis=0))
    cv = np.stack([c[b].reshape(KC, 128).T, c_ctx.reshape(KC, 128).T], axis=-1)
    m["cvec"] = np.ascontiguousarray(cv.astype(np.float32))
    m["ident"] = np.eye(128, dtype=np.float32)
    m["tokidx"] = np.ascontiguousarray(((2 + 16 * g + np.arange(16))[None, :] * 128 + np.arange(128)[:, None]).astype(np.uint32))
    m.update(host_consts())
    m["nabias"] = na_bias_tables(inputs["na_rpb"])
    m["ml_gate_b"] = np.ascontiguousarray(inputs["ml_gate_b"].reshape(DEPTH, 16))
    for k in ("hg_lb", "hg_norm_w", "ml_norm_w"):
        m[k] = np.ascontiguousarray(inputs[k])
    m["moe_router_t"] = np.ascontiguousarray(inputs["moe_router"].transpose(0, 2, 1))
    for k in ("w_branch", "w_out", "ffn_w_up", "ffn_w_down", "moe_w_up", "moe_w_down", "final_norm_w"):
        m[k] = np.ascontiguousarray(inputs[k])
    for k in ("mod_w", "mod_b", "norm1_w", "norm2_w", "w_in"):
        m[k] = np.ascontiguousarray(inputs[k])
    return m


_PROG = None


def kernel(**inputs):
    global _PROG
    inputs = {k: np.asarray(v) for k, v in inputs.items()}
    if _PROG is None:
        _PROG = Prog(upto="all")
        _PROG.build()
    P = _PROG
    names = [n for n in P.dram if n in P.in_names]
    shared = None
    maps = []
    for core in range(8):
        m = host_inputs(inputs, core % 4, core // 4)
        maps.append({k: m[k] for k in names})
    res = run_bass_kernel_spmd(P.nc, maps, core_ids=list(range(8)))
    out = np.empty((4, SEQ, D), np.float32)
    for core in range(8):
        b, g = core % 4, core // 4
        out[b, g * (SEQ // 2):(g + 1) * (SEQ // 2)] = np.asarray(res.results[core]["out"])
    return out
```
